# Optimizing a Trainium2 kernel written in Bass

```python
import jax, jax.numpy as jnp
from jax import lax
import numpy as np

D_MODEL = 1024
BATCH = 4
SEQ = 8192
DEPTH = 2

N_MIXERS = 2
RMS_EPS = 1e-6
ROPE_THETA = 10000.0
NEG_INF = -1e30

MOBA_HEADS = 8
MOBA_HEAD_DIM = 128
MOBA_BLOCK = 256
MOBA_TOPK = 3
MOBA_Q_CHUNK = 64

MLA_HEADS = 8
MLA_Q_RANK = 384
MLA_KV_RANK = 256
MLA_NOPE = 128
MLA_ROPE = 64
MLA_V = 128
MLA_Q_BLOCK = 128

N_GROUPS = 4
EXPERTS_PER_GROUP = 4
N_EXPERTS = N_GROUPS * EXPERTS_PER_GROUP
EXPERT_FF = 256
TOPK_IN_GROUP = 2

PLE_DIM = 256

N_MOBA_LAYERS = (DEPTH + 1) // 2
N_MLA_LAYERS = DEPTH // 2

kernel_name = "hybrid_moba_mla_hmoe_ple"


def rms_norm(x, g):
    xf = x.astype(jnp.float32)
    y = xf * lax.rsqrt(jnp.mean(xf * xf, axis=-1, keepdims=True) + RMS_EPS)
    return (y * g.astype(jnp.float32)).astype(x.dtype)


def rope(x, pos):
    half = x.shape[-1] // 2
    inv = ROPE_THETA ** (-jnp.arange(half, dtype=jnp.float32) / half)
    ang = pos[:, None] * inv[None, :]
    cos, sin = jnp.cos(ang), jnp.sin(ang)
    x1 = x[..., :half].astype(jnp.float32)
    x2 = x[..., half:].astype(jnp.float32)
    out = jnp.concatenate([x1 * cos - x2 * sin, x1 * sin + x2 * cos], axis=-1)
    return out.astype(x.dtype)


def moba_attention(h, w_qkv, w_o):
    B, S, _ = h.shape
    H, Dh, L, QC = MOBA_HEADS, MOBA_HEAD_DIM, MOBA_BLOCK, MOBA_Q_CHUNK
    pos = jnp.arange(S, dtype=jnp.float32)
    qkv = (h @ w_qkv).reshape(B, S, 3, H, Dh).transpose(2, 0, 3, 1, 4)
    q = rope(qkv[0], pos)
    k = rope(qkv[1], pos)
    v = qkv[2]
    nb = -(-S // L)
    pad = nb * L - S
    k_p = jnp.pad(k, ((0, 0), (0, 0), (0, pad), (0, 0)))
    v_p = jnp.pad(v, ((0, 0), (0, 0), (0, pad), (0, 0)))
    k_blk = k_p.reshape(B, H, nb, L, Dh)
    v_blk = v_p.reshape(B, H, nb, L, Dh)
    k_mean = jnp.mean(k_blk.astype(jnp.float32), axis=3)
    k_top = min(MOBA_TOPK, nb)
    scale = Dh ** -0.5
    bi = jnp.arange(B)[:, None, None, None]
    hi = jnp.arange(H)[None, :, None, None]

    def chunk(c):
        start = c * QC
        qc = lax.dynamic_slice_in_dim(q, start, QC, axis=2)
        qpos = start + jnp.arange(QC)
        own = start // L
        gate = jnp.einsum('bhqd,bhnd->bhqn', qc.astype(jnp.float32), k_mean)
        fully_past = jnp.arange(nb)[None, None, None, :] < own
        gate = jnp.where(fully_past, gate, -jnp.inf)
        _, sel = lax.top_k(gate, k_top)
        sel_valid = sel < own
        k_sel = k_blk[bi, hi, sel]
        v_sel = v_blk[bi, hi, sel]
        s_sel = jnp.einsum('bhqd,bhqkld->bhqkl', qc, k_sel).astype(jnp.float32) * scale
        s_sel = jnp.where(sel_valid[..., None], s_sel, NEG_INF).reshape(B, H, QC, k_top * L)
        k_own = lax.dynamic_slice_in_dim(k_p, own * L, L, axis=2)
        v_own = lax.dynamic_slice_in_dim(v_p, own * L, L, axis=2)
        s_own = jnp.einsum('bhqd,bhld->bhql', qc, k_own).astype(jnp.float32) * scale
        kpos = own * L + jnp.arange(L)
        s_own = jnp.where(kpos[None, :] <= qpos[:, None], s_own, NEG_INF)
        pr = jax.nn.softmax(jnp.concatenate([s_sel, s_own], axis=-1), axis=-1)
        p_sel = pr[..., :k_top * L].reshape(B, H, QC, k_top, L).astype(v.dtype)
        p_own = pr[..., k_top * L:].astype(v.dtype)
        return (jnp.einsum('bhqkl,bhqkld->bhqd', p_sel, v_sel)
                + jnp.einsum('bhql,bhld->bhqd', p_own, v_own))

    o = lax.map(chunk, jnp.arange(S // QC))
    o = o.transpose(1, 0, 3, 2, 4).reshape(B, S, H * Dh)
    return o @ w_o


def mla_attention(h, w_dq, q_norm, w_uq, w_dkv, kv_norm, w_ukv, w_o):
    B, S, _ = h.shape
    H, QB = MLA_HEADS, MLA_Q_BLOCK
    pos = jnp.arange(S, dtype=jnp.float32)
    cq = rms_norm(h @ w_dq, q_norm)
    q = (cq @ w_uq).reshape(B, S, H, MLA_NOPE + MLA_ROPE).transpose(0, 2, 1, 3)
    q_nope = q[..., :MLA_NOPE]
    q_rope = rope(q[..., MLA_NOPE:], pos)
    kv_a = h @ w_dkv
    ckv = rms_norm(kv_a[..., :MLA_KV_RANK], kv_norm)
    k_rope = rope(kv_a[..., MLA_KV_RANK:], pos)
    kv = (ckv @ w_ukv).reshape(B, S, H, MLA_NOPE + MLA_V).transpose(0, 2, 1, 3)
    k_nope = kv[..., :MLA_NOPE]
    v = kv[..., MLA_NOPE:]
    scale = (MLA_NOPE + MLA_ROPE) ** -0.5
    kpos = jnp.arange(S)

    def qblock(c):
        start = c * QB
        qn = lax.dynamic_slice_in_dim(q_nope, start, QB, axis=2)
        qr = lax.dynamic_slice_in_dim(q_rope, start, QB, axis=2)
        s = (jnp.einsum('bhqd,bhkd->bhqk', qn, k_nope)
             + jnp.einsum('bhqd,bkd->bhqk', qr, k_rope)).astype(jnp.float32) * scale
        qpos = start + jnp.arange(QB)
        s = jnp.where(kpos[None, :] <= qpos[:, None], s, NEG_INF)
        pr = jax.nn.softmax(s, axis=-1).astype(v.dtype)
        return jnp.einsum('bhqk,bhkd->bhqd', pr, v)

    o = lax.map(qblock, jnp.arange(S // QB))
    o = o.transpose(1, 0, 3, 2, 4).reshape(B, S, H * MLA_V)
    return o @ w_o


def hier_moe(h, w_group, w_expert, w_gate, w_up, w_down):
    B, S, _ = h.shape
    hf = h.astype(jnp.float32)
    g_logits = hf @ w_group.astype(jnp.float32)
    g_prob = jax.nn.softmax(g_logits, axis=-1)
    g_sel = jnp.argmax(g_logits, axis=-1)
    g_w = jnp.take_along_axis(g_prob, g_sel[..., None], axis=-1)
    e_logits = (hf @ w_expert.astype(jnp.float32)).reshape(B, S, N_GROUPS, EXPERTS_PER_GROUP)
    e_in = jnp.take_along_axis(e_logits, g_sel[..., None, None], axis=2)[..., 0, :]
    top_v, top_i = lax.top_k(e_in, TOPK_IN_GROUP)
    top_w = jax.nn.softmax(top_v, axis=-1) * g_w
    expert_id = g_sel[..., None] * EXPERTS_PER_GROUP + top_i
    combine = jnp.sum(jax.nn.one_hot(expert_id, N_EXPERTS, dtype=jnp.float32) * top_w[..., None], axis=-2)
    a = jnp.einsum('bsd,edf->bsef', h, w_gate)
    u = jnp.einsum('bsd,edf->bsef', h, w_up)
    act = jax.nn.silu(a) * u * combine.astype(h.dtype)[..., None]
    return jnp.einsum('bsef,efd->bsd', act, w_down)


def setup_inputs(seed: int = 0) -> dict:
    key = jax.random.key(seed)
    ks = iter(jax.random.split(key, 32))
    f32 = jnp.float32

    def w(shape, fan_in):
        return jax.random.normal(next(ks), shape, f32) * (fan_in ** -0.5)

    def gain(shape):
        return 1.0 + 0.02 * jax.random.normal(next(ks), shape, f32)

    D = D_MODEL
    return {
        "x": jax.random.normal(next(ks), (BATCH, SEQ, D), f32),
        "p": jax.random.normal(next(ks), (DEPTH, BATCH, SEQ, PLE_DIM), f32),
        "attn_norm": gain((DEPTH, D)),
        "ffn_norm": gain((DEPTH, D)),
        "ple_norm": gain((DEPTH, D)),
        "final_norm": gain((D,)),
        "moba_wqkv": w((N_MOBA_LAYERS, D, 3 * MOBA_HEADS * MOBA_HEAD_DIM), D),
        "moba_wo": w((N_MOBA_LAYERS, MOBA_HEADS * MOBA_HEAD_DIM, D), MOBA_HEADS * MOBA_HEAD_DIM),
        "mla_wdq": w((N_MLA_LAYERS, D, MLA_Q_RANK), D),
        "mla_qnorm": gain((N_MLA_LAYERS, MLA_Q_RANK)),
        "mla_wuq": w((N_MLA_LAYERS, MLA_Q_RANK, MLA_HEADS * (MLA_NOPE + MLA_ROPE)), MLA_Q_RANK),
        "mla_wdkv": w((N_MLA_LAYERS, D, MLA_KV_RANK + MLA_ROPE), D),
        "mla_kvnorm": gain((N_MLA_LAYERS, MLA_KV_RANK)),
        "mla_wukv": w((N_MLA_LAYERS, MLA_KV_RANK, MLA_HEADS * (MLA_NOPE + MLA_V)), MLA_KV_RANK),
        "mla_wo": w((N_MLA_LAYERS, MLA_HEADS * MLA_V, D), MLA_HEADS * MLA_V),
        "moe_wgroup": w((DEPTH, D, N_GROUPS), D),
        "moe_wexpert": w((DEPTH, D, N_EXPERTS), D),
        "moe_wgate": w((DEPTH, N_EXPERTS, D, EXPERT_FF), D),
        "moe_wup": w((DEPTH, N_EXPERTS, D, EXPERT_FF), D),
        "moe_wdown": w((DEPTH, N_EXPERTS, EXPERT_FF, D), EXPERT_FF),
        "ple_gate": w((DEPTH, D, D), D),
        "ple_proj": w((DEPTH, PLE_DIM, D), PLE_DIM),
    }


def reference(x, p, attn_norm, ffn_norm, ple_norm, final_norm, moba_wqkv, moba_wo,
              mla_wdq, mla_qnorm, mla_wuq, mla_wdkv, mla_kvnorm, mla_wukv, mla_wo,
              moe_wgroup, moe_wexpert, moe_wgate, moe_wup, moe_wdown, ple_gate, ple_proj):
    h = x
    for i in range(DEPTH):
        hn = rms_norm(h, attn_norm[i])
        j = i // N_MIXERS
        if i % N_MIXERS == 0:
            mix = moba_attention(hn, moba_wqkv[j], moba_wo[j])
        else:
            mix = mla_attention(hn, mla_wdq[j], mla_qnorm[j], mla_wuq[j], mla_wdkv[j],
                                mla_kvnorm[j], mla_wukv[j], mla_wo[j])
        h = h + mix
        h = h + hier_moe(rms_norm(h, ffn_norm[i]), moe_wgroup[i], moe_wexpert[i],
                         moe_wgate[i], moe_wup[i], moe_wdown[i])
        gate = jax.nn.sigmoid(rms_norm(h, ple_norm[i]) @ ple_gate[i])
        h = h + gate * (p[i] @ ple_proj[i])
    return rms_norm(h, final_norm)
```

```python
import numpy as np
import ml_dtypes
from contextlib import ExitStack
import concourse.bass as bass
import concourse.mybir as mybir
from concourse.bass_utils import run_bass_kernel_spmd

F32, BF16 = mybir.dt.float32, mybir.dt.bfloat16
AF = mybir.ActivationFunctionType
ALU = mybir.AluOpType
AX = mybir.AxisListType
BIG = 30000.0
NEG = -1.0e30
T = 512
NL = 4096
NG = 8192
NJ = 8
NGC = 16
RMS_EPS = 1e-6
NPBF = ml_dtypes.bfloat16


class KB:
    def __init__(self, nc, es):
        self.nc = nc
        self.h = {'pe': nc.tensor, 'act': nc.scalar, 'dve': nc.vector, 'pool': nc.gpsimd, 'sp': nc.sync}
        self.csem = {e: es.enter_context(nc.semaphore("c_" + e)) for e in ('pe', 'act', 'dve', 'pool')}
        self.cnt = {e: 0 for e in self.csem}
        NQ = 8
        self.dsem = {q: [es.enter_context(nc.semaphore(f"d_{q}{i}")) for i in range(NQ)] for q in ('sp', 'pool')}
        self.dval = {q: [0] * NQ for q in self.dsem}
        self.dnext = {q: 0 for q in self.dsem}
        self.waited = {e: {} for e in self.h}
        self.lastw = {}
        self.readers = {}

    def _wait(self, eng, tok):
        semkey, sem, val, src = tok
        if self.waited[eng].get(semkey, 0) >= val:
            return
        self.h[eng].wait_ge(sem, val)
        self.waited[eng][semkey] = val

    def _deps(self, eng, reads, writes):
        for k in reads:
            t = self.lastw.get(k)
            if t is not None and not (t[3] == 'pe' and eng == 'pe'):
                self._wait(eng, t)
        for k in writes:
            t = self.lastw.get(k)
            if t is not None and not (t[3] == 'pe' and eng == 'pe'):
                self._wait(eng, t)
            for t in self.readers.get(k, ()):
                if t[3] != eng:
                    self._wait(eng, t)

    def _record(self, tok, reads, writes):
        for k in writes:
            self.lastw[k] = tok
            self.readers[k] = []
        for k in reads:
            self.readers.setdefault(k, []).append(tok)

    def op(self, eng, fn, reads=(), writes=()):
        self._deps(eng, reads, writes)
        inst = fn(self.h[eng])
        self.cnt[eng] += 1
        inst.then_inc(self.csem[eng], 1)
        tok = ('c' + eng, self.csem[eng], self.cnt[eng], eng)
        self._record(tok, reads, writes)
        return tok

    def dma(self, q, out, in_, reads=(), writes=()):
        slot = self.dnext[q]
        self.dnext[q] = (slot + 1) % len(self.dsem[q])
        sem = self.dsem[q][slot]
        prev = ('d' + q + str(slot), sem, self.dval[q][slot], 'dma')
        if prev[2] > 0:
            self._wait(q, prev)
        self._deps(q, reads, writes)
        self.h[q].dma_start(out=out, in_=in_).then_inc(sem, 16)
        self.dval[q][slot] += 16
        tok = ('d' + q + str(slot), sem, self.dval[q][slot], 'dma')
        self._record(tok, reads, writes)
        return tok

    def barrier(self):
        toks = []
        for e in self.csem:
            if self.cnt[e] > 0:
                toks.append(('c' + e, self.csem[e], self.cnt[e], e))
        for q in self.dsem:
            for i, s in enumerate(self.dsem[q]):
                if self.dval[q][i] > 0:
                    toks.append(('d' + q + str(i), s, self.dval[q][i], 'dma'))
        for e in self.h:
            for t in toks:
                if t[3] == e:
                    continue
                self._wait(e, t)
        self.lastw = {}
        self.readers = {}


def build(mode='fused'):
    nc = bass.Bass("TRN2", target_bir_lowering=False)

    def din(name, shape, dt=F32):
        return nc.dram_tensor(name, list(shape), dt, kind="ExternalInput").ap()

    def dout(name, shape, dt=F32):
        return nc.dram_tensor(name, list(shape), dt, kind="ExternalOutput").ap()

    def dint(name, shape, dt=F32):
        return nc.dram_tensor(name, list(shape), dt, kind="Internal").ap()

    I = {}
    I['gains'] = din('gains', [128, 7, 8])
    I['ones_bf'] = din('ones_bf', [128, 128], BF16)
    I['ident_bf'] = din('ident_bf', [128, 128], BF16)
    I['ident_f'] = din('ident_f', [128, 128])
    I['sel16'] = din('sel16', [16, 16, 128], BF16)
    I['sel2'] = din('sel2', [128, 2])
    I['xT_all'] = din('xT_all', [1024, NG])
    I['p0T'] = din('p0T', [256, NG])
    I['p1T'] = din('p1T', [256, NL])
    I['cosg'] = din('cosg', [128, NG])
    I['sing'] = din('sing', [128, NG])
    I['cos64g'] = din('cos64g', [128, NG])
    I['sin64g'] = din('sin64g', [128, NG])
    I['cos64'] = din('cos64', [128, NL])
    I['sin64'] = din('sin64', [128, NL])
    for n in ('wq0', 'wq0s', 'wk0', 'wk0s', 'wv0', 'wo0', 'wo1'):
        I[n] = din(n, [1024, 1024])
    I['cbias'] = din('cbias', [128, NGC * 4 * 32])
    I['aown'] = din('aown', [128, NGC * 4 * 32])
    I['btab'] = din('btab', [128, NGC * 4 * 32])
    I['selT'] = din('selT', [32, 32, 128], BF16)
    I['dm0'] = din('dm0', [128, 4, T], BF16)
    I['dm1'] = din('dm1', [128, 8, T], BF16)
    I['wdq'] = din('wdq', [1024, 384])
    I['wuqn'] = din('wuqn', [384, 1024])
    I['wuqr'] = din('wuqr', [384, 512])
    I['wuqrs'] = din('wuqrs', [384, 512])
    I['wdkvc'] = din('wdkvc', [1024, 256])
    I['wdkvr'] = din('wdkvr', [1024, 64])
    I['wdkvrs'] = din('wdkvrs', [1024, 64])
    I['qn'] = din('qn', [128, 3])
    I['kvn'] = din('kvn', [128, 2])
    I['wukvk'] = din('wukvk', [256, 1024])
    I['wukvv'] = din('wukvv', [256, 1024])
    for li in (0, 1):
        I[f'wr{li}'] = din(f'wr{li}', [1024, 20])
        I[f'wg{li}'] = din(f'wg{li}', [16, 1024, 256])
        I[f'wu{li}'] = din(f'wu{li}', [16, 1024, 256])
        I[f'wd{li}'] = din(f'wd{li}', [16, 256, 1024])
        I[f'pg{li}'] = din(f'pg{li}', [1024, 1024])
        I[f'pp{li}'] = din(f'pp{li}', [256, 1024])

    hT_all = dint('hT_all', [1024, NG], F32)
    hT_loc = dint('hT_loc', [1024, NL], F32)
    qnT = dint('qnT', [1024, NL], BF16)
    qrT = dint('qrT', [512, NL], BF16)
    kT = dint('kT', [1024, NG], BF16)
    krT = dint('krT', [64, NG], BF16)
    vdr = dint('vdr', [8, 128, 64, 128], BF16)
    qT0 = dint('qT0', [1024, NG], BF16)
    oT = dint('oT', [1024, NG], BF16)
    outT = dout('outT', [1024, NL], F32)

    es = ExitStack()
    with es:
        kb = KB(nc, es)

        uid = [0]

        def uname(name):
            uid[0] += 1
            return f"s{uid[0]}_{name}"

        def sb(name, shape, dt):
            return es.enter_context(nc.sbuf_tensor(uname(name), list(shape), dt))

        gains = sb('gains', [128, 7, 8], F32)
        ones_bf = sb('ones_bf', [128, 128], BF16)
        ident_bf = sb('ident_bf', [128, 128], BF16)
        ident_f = sb('ident_f', [128, 128], F32)
        sel16 = sb('sel16', [16, 16, 128], BF16)
        kb.dma('sp', gains[:], I['gains'], writes=['gains'])
        kb.dma('sp', ones_bf[:], I['ones_bf'], writes=['ones_bf'])
        kb.dma('sp', ident_bf[:], I['ident_bf'], writes=['ident_bf'])
        kb.dma('sp', ident_f[:], I['ident_f'], writes=['ident_f'])
        kb.dma('sp', sel16[:], I['sel16'], writes=['sel16'])
        CONSTK = ['gains', 'ones_bf', 'ident_bf', 'ident_f', 'sel16']

        ps = [es.enter_context(nc.psum_tensor(f"ps{i}", [128, T], F32)) for i in range(7)]
        psb = es.enter_context(nc.psum_tensor("psb", [128, 2 * T], BF16))

        stg = [sb(f'stg{i}', [128, 1024], F32) for i in range(2)]
        stg_i = [0]

        def load_w(dst, dkey, src, kc_n, N, rows=128, ceng='pool'):
            for kc in range(kc_n):
                for n0 in range(0, N, 1024):
                    nn = min(1024, N - n0)
                    i = stg_i[0]
                    stg_i[0] = (i + 1) % 2
                    kb.dma('sp', stg[i][:rows, :nn], src[kc * rows:(kc + 1) * rows, n0:n0 + nn],
                           writes=[f'stg{i}'])
                    if ceng == 'act':
                        kb.op('act', lambda e, i=i, kc=kc, n0=n0, nn=nn: e.activation(
                            out=dst[:rows, kc, n0:n0 + nn], in_=stg[i][:rows, :nn], func=AF.Copy),
                            reads=[f'stg{i}'], writes=[dkey])
                    else:
                        kb.op('pool', lambda e, i=i, kc=kc, n0=n0, nn=nn: e.tensor_copy(
                            out=dst[:rows, kc, n0:n0 + nn], in_=stg[i][:rows, :nn]),
                            reads=[f'stg{i}'], writes=[dkey])

        sq = sb('sq', [128, 8, T], BF16)
        rs = sb('rs', [128, T], F32)
        lnv = sb('lnv', [128, T], F32)

        rs_b = sb('rs_b', [128, T], F32)
        rsl = [rs, rs_b]

        def rmsnorm_p1(src_fn, skey, nch, Dn, psbank, ri=0):
            for c in range(nch):
                kb.op('act', lambda e, c=c: e.activation(out=sq[:, c, :], in_=src_fn(c), func=AF.Square),
                      reads=[skey], writes=['sq'])
            for c in range(nch):
                kb.op('pe', lambda e, c=c: e.matmul(ps[psbank][:], lhsT=ones_bf[:], rhs=sq[:, c, :],
                                                    start=(c == 0), stop=(c == nch - 1)),
                      reads=['sq', 'ones_bf'], writes=[f'ps{psbank}'])
            kb.op('act', lambda e: e.activation(out=lnv[:], in_=ps[psbank][:], func=AF.Ln,
                                                scale=1.0 / Dn, bias=eps_t[:, 0:1]),
                  reads=[f'ps{psbank}', 'eps'], writes=['lnv'])
            kb.op('act', lambda e: e.activation(out=rsl[ri][:], in_=lnv[:], func=AF.Exp, scale=-0.5),
                  reads=['lnv'], writes=[f'rs{ri}'])

        def rmsnorm_p2(src_fn, skey, nch, gcol_fn, dst_fn, dkey, ri=0, dst_f_fn=None, dfkey=None):
            for c in range(nch):
                if dst_f_fn is not None:
                    kb.op('dve', lambda e, c=c: e.scalar_tensor_tensor(
                        out=dst_f_fn(c), in0=src_fn(c), scalar=gcol_fn(c), in1=rsl[ri][:],
                        op0=ALU.mult, op1=ALU.mult), reads=[skey, f'rs{ri}', 'gains', 'qkvn'],
                        writes=[dfkey, f'{dfkey}_{c}'])
                    kb.op('pool', lambda e, c=c: e.tensor_copy(out=dst_fn(c), in_=dst_f_fn(c)),
                          reads=[f'{dfkey}_{c}'], writes=[dkey])
                else:
                    kb.op('dve', lambda e, c=c: e.scalar_tensor_tensor(
                        out=dst_fn(c), in0=src_fn(c), scalar=gcol_fn(c), in1=rsl[ri][:],
                        op0=ALU.mult, op1=ALU.mult), reads=[skey, f'rs{ri}', 'gains', 'qkvn'], writes=[dkey])

        def rmsnorm(src_fn, skey, nch, gcol_fn, dst_fn, dkey, Dn, psbank, dst_f_fn=None, dfkey=None):
            rmsnorm_p1(src_fn, skey, nch, Dn, psbank, 0)
            rmsnorm_p2(src_fn, skey, nch, gcol_fn, dst_fn, dkey, 0, dst_f_fn, dfkey)

        eps_t = sb('eps_t', [128, 1], F32)
        kb.op('dve', lambda e: e.memset(eps_t[:], RMS_EPS), writes=['eps'])

        def attention(layer, scale, dm_sb, es2):
            def sb2(name, shape, dt):
                return es2.enter_context(nc.sbuf_tensor(uname(name), list(shape), dt))
            mla = layer == 1
            NQT = NJ if mla else NGC
            kTh = [sb2(f'kTh{i}', [128, NG], BF16) for i in range(2)]
            vh = [sb2(f'vh{i}', [128, 64, 128], BF16) for i in range(2)]
            qTh = [sb2(f'qTh{i}', [128, NQT * T], BF16) for i in range(2)]
            if mla:
                krs = sb2('krs', [128, NG], BF16)
                qrh = [sb2(f'qrh{i}', [128, NL], BF16) for i in range(2)]
                kb.op('pool', lambda e: e.memset(krs[64:128, :], 0.0), writes=['krs'])
                for i in range(2):
                    kb.op('pool', lambda e, i=i: e.memset(qrh[i][64:128, :], 0.0), writes=[f'qrh{i}'])
                kb.dma('sp', krs[0:64, :], krT, reads=['krT'], writes=['krs'])
            else:
                fm = sb2('fm', [128, 4, 32], BF16)
                gm = sb2('gm', [128, 4, 32], F32)
                selm = sb2('selm', [128, 4, 32], F32)
                m8 = sb2('m8', [128, 4, 8], F32)
                fmT = [sb2(f'fmT{i}', [128, T], BF16) for i in range(2)]
                for i in range(2):
                    kb.op('pool', lambda e, i=i: e.memset(fmT[i][:], 0.0), writes=[f'fmT{i}'])
            pT = [sb2(f'pT{i}', [128, T], BF16) for i in range(2)]
            rinv = sb2('rinv', [128, T], F32)
            ost = [sb2(f'ost{i}', [128, T], BF16) for i in range(2)]

            def load_head(h):
                hb = h % 2
                for part in range(4):
                    kb.dma('sp', kTh[hb][:, part * 2048:(part + 1) * 2048],
                           kT[h * 128:(h + 1) * 128, part * 2048:(part + 1) * 2048],
                           reads=['kT'], writes=[f'kTh{hb}'])
                for part in range(4):
                    kb.dma('sp', vh[hb][:, part * 16:(part + 1) * 16, :], vdr[h, :, part * 16:(part + 1) * 16, :],
                           reads=['vdr'], writes=[f'vh{hb}'])
                qsrc = qnT if mla else qT0
                kb.dma('sp', qTh[hb][:], qsrc[h * 128:(h + 1) * 128, :], reads=['qsrc'], writes=[f'qTh{hb}'])
                if mla:
                    kb.dma('sp', qrh[hb][0:64, :], qrT[h * 64:(h + 1) * 64, :], reads=['qrT'], writes=[f'qrh{hb}'])

            def geom(j):
                nkt = 4 * (2 * j + 2) if mla else 4 * (j + 1)
                dstart = 8 * j if mla else 4 * j
                return nkt, dstart

            def qk(h, j, ob, kt):
                hb = h % 2
                nkt, dstart = geom(j)
                qs = slice(j * T, (j + 1) * T)
                sbk = kt % 2
                diag = kt >= dstart
                ks = slice(kt * 128, (kt + 1) * 128)
                kb.op('pe', lambda e: e.matmul(ps[sbk][:], lhsT=kTh[hb][:, ks], rhs=qTh[hb][:, qs],
                                               start=True, stop=False),
                      reads=[f'kTh{hb}', f'qTh{hb}'], writes=[f'ps{sbk}'])
                if mla:
                    kb.op('pe', lambda e: e.matmul(ps[sbk][:], lhsT=krs[:, ks], rhs=qrh[hb][:, qs],
                                                   start=False, stop=not diag),
                          reads=['krs', f'qrh{hb}'], writes=[f'ps{sbk}'])
                else:
                    kb.op('pe', lambda e: e.matmul(ps[sbk][:], lhsT=selT[:, kt // 2, :], rhs=fmT[ob][:],
                                                   start=False, stop=not diag),
                          reads=['selT', f'fmT{ob}'], writes=[f'ps{sbk}'])
                if diag:
                    kb.op('pe', lambda e: e.matmul(ps[sbk][:], lhsT=ident_bf[:], rhs=dm_sb[:, kt - dstart, :],
                                                   start=False, stop=True),
                          reads=['ident_bf', 'dm'], writes=[f'ps{sbk}'])

            def prologue_a(h, j, ob):
                hb = h % 2
                if mla:
                    return
                for s in range(4):
                    kb.op('pe', lambda e, s=s: e.matmul(
                        ps[6][:, s * 32:(s + 1) * 32], lhsT=qTh[hb][:, j * T + s * 128: j * T + (s + 1) * 128],
                        rhs=kmT[:, h, :], start=True, stop=True),
                        reads=[f'qTh{hb}', 'kmT'], writes=['ps6'])
                tb = slice(j * 128, (j + 1) * 128)
                fl = "p a b -> p (a b)"
                kb.op('dve', lambda e: e.tensor_tensor(
                    out=gm[:].rearrange(fl), in0=ps[6][:, 0:128], in1=cbias[:, tb], op=ALU.add),
                    reads=['ps6', 'tabs'], writes=['gm'])
                for s in range(4):
                    kb.op('dve', lambda e, s=s: e.max(out=m8[:, s, :], in_=gm[:, s, :]),
                          reads=['gm'], writes=['m8'])
                for s in range(4):
                    kb.op('dve', lambda e, s=s: e.tensor_scalar(
                        out=selm[:, s, :], in0=gm[:, s, :], scalar1=m8[:, s, 2:3], scalar2=None,
                        op0=ALU.is_ge), reads=['gm', 'm8'], writes=['selm'])
                kb.op('dve', lambda e: e.tensor_tensor(
                    out=selm[:].rearrange(fl), in0=selm[:].rearrange(fl),
                    in1=aown[:, tb], op=ALU.max), reads=['selm', 'tabs'], writes=['selm'])
                kb.op('dve', lambda e: e.tensor_scalar(
                    out=selm[:].rearrange(fl), in0=selm[:].rearrange(fl),
                    scalar1=1.0, scalar2=BIG, op0=ALU.subtract, op1=ALU.mult),
                    reads=['selm'], writes=['selm'])
                kb.op('dve', lambda e: e.tensor_tensor(
                    out=fm[:].rearrange(fl), in0=selm[:].rearrange(fl),
                    in1=btab[:, tb], op=ALU.add), reads=['selm', 'tabs'], writes=['fm'])

            def prologue_b(h, j, ob):
                if mla:
                    return
                for s in range(4):
                    kb.op('pe', lambda e, s=s: e.transpose(
                        out=psb[0:32, s * 128:(s + 1) * 128], in_=fm[:, s, :], identity=ident_bf[:]),
                        reads=['fm', 'ident_bf'], writes=['psb'])
                kb.op('dve', lambda e: e.tensor_copy(out=fmT[ob][0:32, :], in_=psb[0:32, 0:T]),
                      reads=['psb'], writes=[f'fmT{ob}'])

            def mainloop(h, j, ob, nxt):
                hb = h % 2
                nkt, dstart = geom(j)
                po, pr = ps[2 + ob], ps[4 + ob]
                pok, prk = f'ps{2 + ob}', f'ps{4 + ob}'
                ka = max(0, nkt - 9)
                kbb = max(ka + 1, nkt - 3) if nkt > 4 else nkt - 1
                def pv(kt):
                    sbk = kt % 2
                    kb.op('pe', lambda e: e.matmul(po[:], lhsT=vh[hb][:, kt, :], rhs=pT[sbk][:],
                                                   start=(kt == 0), stop=(kt == nkt - 1)),
                          reads=[f'vh{hb}', f'pT{sbk}'], writes=[pok])
                    kb.op('pe', lambda e: e.matmul(pr[:], lhsT=ones_bf[:], rhs=pT[sbk][:],
                                                   start=(kt == 0), stop=(kt == nkt - 1)),
                          reads=['ones_bf', f'pT{sbk}'], writes=[prk])

                for kt in range(nkt):
                    sbk = kt % 2
                    if nxt is not None and kt == ka:
                        prologue_a(*nxt)
                    if nxt is not None and kt == kbb:
                        prologue_b(*nxt)
                    if kt + 1 < nkt:
                        qk(h, j, ob, kt + 1)
                    kb.op('act', lambda e, sbk=sbk: e.activation(out=pT[sbk][:], in_=ps[sbk][:], func=AF.Exp,
                                                                 scale=scale),
                          reads=[f'ps{sbk}'], writes=[f'pT{sbk}'])
                    if kt >= 1:
                        pv(kt - 1)
                return lambda: pv(nkt - 1)

            def epilogue(h, j, ob):
                po, pr = ps[2 + ob], ps[4 + ob]
                pok, prk = f'ps{2 + ob}', f'ps{4 + ob}'
                qs = slice(j * T, (j + 1) * T)
                kb.op('dve', lambda e: e.reciprocal(out=rinv[:], in_=pr[:]), reads=[prk], writes=['rinv'])
                kb.op('dve', lambda e: e.tensor_tensor(out=ost[ob][:], in0=po[:], in1=rinv[:], op=ALU.mult),
                      reads=[pok, 'rinv'], writes=[f'ost{ob}'])
                kb.dma('pool', oT[h * 128:(h + 1) * 128, qs], ost[ob][:], reads=[f'ost{ob}'], writes=['oT'])

            items = [(h, j) for h in range(8) for j in range(NQT)]
            load_head(0)
            prologue_a(items[0][0], items[0][1], 0)
            prologue_b(items[0][0], items[0][1], 0)
            qk(items[0][0], items[0][1], 0, 0)
            for idx, (h, j) in enumerate(items):
                ob = idx % 2
                if j == 0 and h + 1 < 8:
                    load_head(h + 1)
                nxt = None
                if idx + 1 < len(items):
                    nxt = (items[idx + 1][0], items[idx + 1][1], (idx + 1) % 2)
                finish = mainloop(h, j, ob, nxt)
                if nxt is not None:
                    qk(nxt[0], nxt[1], nxt[2], 0)
                finish()
                epilogue(h, j, ob)

        def rowlocal(layer, h_src, wo_ap, pT_ap, es2, nst, h_dst):
            def sb2(name, shape, dt):
                return es2.enter_context(nc.sbuf_tensor(uname(name), list(shape), dt))
            ST = 2048
            hs = sb2('hs', [128, 8, ST], F32)
            a32 = sb2('a32', [128, 8, ST], BF16)
            wbig = sb2('wbig', [128, 8, 1024], BF16)
            wpp = sb2('wpp', [128, 2, 1024], BF16)
            wgs = [sb2(f'wgs{i}', [128, 8, 256], BF16) for i in range(2)]
            wus = [sb2(f'wus{i}', [128, 8, 256], BF16) for i in range(2)]
            wds = [sb2(f'wds{i}', [128, 2, 1024], BF16) for i in range(2)]
            wr = sb2('wr', [128, 8, 20], F32)
            xnf = sb2('xnf', [128, 8, T], F32)
            combT = sb2('combT', [16, ST], BF16)
            lg = sb2('lg', [128, 20], F32)
            sm = sb2('sm', [128, 64], F32)
            em = sb2('em', [128, 16], F32)
            ee = sb2('ee', [128, 20], F32)
            selr = sb2('selr', [128, 16], F32)
            combs = [sb2(f'comb{i}', [128, 16], F32) for i in range(2)]
            m8r = sb2('m8r', [128, 8], F32)
            sa = [sb2(f'sa{i}', [128, T], F32) for i in range(2)]
            tt = [sb2(f'tt{i}', [128, T], F32) for i in range(2)]
            actb = [sb2(f'actb{i}', [128, 2, T], BF16) for i in range(2)]
            pf = sb2('pf', [128, 2, T], F32)
            ytmp = [pf[:, 0, :], pf[:, 1, :]]
            pbf = sb2('pbf', [128, 2, T], BF16)
            of = xnf
            kb.dma('sp', wr[:], I[f'wr{layer}'].rearrange("(c p) n -> p c n", p=128), writes=['wr'])
            ga, gf, gp = layer, 2 + layer, 4 + layer
            for st in range(nst):
                ts0 = st * ST
                for c in range(8):
                    kb.dma('sp', a32[:, c, :], oT[c * 128:(c + 1) * 128, ts0:ts0 + ST], reads=['oT'], writes=['a32'])
                load_w(wbig, 'wbig', wo_ap, 8, 1024, ceng='act')
                for c in range(8):
                    kb.dma('sp', hs[:, c, :], h_src[c * 128:(c + 1) * 128, ts0:ts0 + ST], reads=['hsrc'], writes=['hs'])
                n = 0
                for sub in range(4):
                    ss = slice(sub * T, (sub + 1) * T)
                    for oc in range(8):
                        b = n % 2
                        n += 1
                        for kc in range(8):
                            kb.op('pe', lambda e, kc=kc, oc=oc, b=b: e.matmul(
                                ps[b][:], lhsT=wbig[:, kc, oc * 128:(oc + 1) * 128], rhs=a32[:, kc, ss],
                                start=(kc == 0), stop=(kc == 7)), reads=['wbig', 'a32'], writes=[f'ps{b}'])
                        kb.op('dve', lambda e, oc=oc, b=b: e.tensor_tensor(
                            out=hs[:, oc, ss], in0=ps[b][:], in1=hs[:, oc, ss], op=ALU.add),
                            reads=[f'ps{b}', 'hs'], writes=['hs'])
                def fnorm_p1(sub):
                    ss = slice(sub * T, (sub + 1) * T)
                    rmsnorm_p1(lambda c: hs[:, c, ss], 'hs', 8, 1024.0, 6, sub % 2)

                def fnorm_p2(sub):
                    ss = slice(sub * T, (sub + 1) * T)
                    rmsnorm_p2(lambda c: hs[:, c, ss], 'hs', 8, lambda c: gains[:, gf, c:c + 1],
                               lambda c: a32[:, c, ss], 'a32', sub % 2,
                               dst_f_fn=lambda c: xnf[:, c, :], dfkey='xnf')

                LB = [(ps[5], 'ps5'), (ps[2], 'ps2')]
                TB = [(ps[4], 'ps4'), (ps[3], 'ps3')]

                def router_logits(s4):
                    lb, lk = LB[s4 % 2]
                    for kc in range(8):
                        kb.op('pe', lambda e, kc=kc: e.matmul(
                            lb[:, 0:20], lhsT=xnf[:, kc, s4 * 128:(s4 + 1) * 128], rhs=wr[:, kc, :],
                            start=(kc == 0), stop=(kc == 7)), reads=['xnf', 'wr'], writes=[lk])

                def router(sub):
                    router_logits(0)
                    for s4 in range(4):
                        if s4 + 1 < 4:
                            router_logits(s4 + 1)
                        lb, lk = LB[s4 % 2]
                        tb_, tk = TB[s4 % 2]
                        comb = combs[s4 % 2]
                        ck = f'comb{s4 % 2}'
                        D = 'dve'
                        kb.op(D, lambda e: e.tensor_copy(out=lg[:], in_=lb[:, 0:20]), reads=[lk], writes=['lg'])
                        kb.op(D, lambda e: e.tensor_reduce(out=sm[:, 0:1], in_=lg[:, 0:4], axis=AX.X, op=ALU.max),
                              reads=['lg'], writes=['sm'])
                        kb.op(D, lambda e: e.tensor_scalar(out=ee[:, 0:4], in0=lg[:, 0:4], scalar1=sm[:, 0:1],
                                                           scalar2=None, op0=ALU.subtract), reads=['lg', 'sm'], writes=['ee'])
                        kb.op(D, lambda e: e.tensor_scalar(out=sm[:, 12:16], in0=lg[:, 0:4], scalar1=sm[:, 0:1],
                                                           scalar2=None, op0=ALU.is_ge), reads=['lg', 'sm'], writes=['sm'])
                        kb.op(D, lambda e: e.tensor_scalar(out=sm[:, 12:16], in0=sm[:, 12:16], scalar1=1.0, scalar2=1e30,
                                                           op0=ALU.subtract, op1=ALU.mult), reads=['sm'], writes=['sm'])
                        for g in range(4):
                            kb.op(D, lambda e, g=g: e.tensor_scalar(
                                out=em[:, 4 * g:4 * g + 4], in0=lg[:, 4 + 4 * g:8 + 4 * g], scalar1=sm[:, 12 + g:13 + g],
                                scalar2=None, op0=ALU.add), reads=['lg', 'sm'], writes=['em'])
                        kb.op(D, lambda e: e.max(out=m8r[:], in_=em[:]), reads=['em'], writes=['m8r'])
                        kb.op(D, lambda e: e.tensor_scalar(out=selr[:], in0=em[:], scalar1=m8r[:, 1:2], scalar2=None,
                                                           op0=ALU.is_ge), reads=['em', 'm8r'], writes=['selr'])
                        kb.op(D, lambda e: e.tensor_scalar(out=ee[:, 4:20], in0=em[:], scalar1=m8r[:, 0:1], scalar2=-80.0,
                                                           op0=ALU.subtract, op1=ALU.max), reads=['em', 'm8r'], writes=['ee'])
                        kb.op('act', lambda e: e.activation(out=ee[:], in_=ee[:], func=AF.Exp), reads=['ee'], writes=['ee'])
                        kb.op(D, lambda e: e.tensor_reduce(out=sm[:, 1:2], in_=ee[:, 0:4], axis=AX.X, op=ALU.add),
                              reads=['ee'], writes=['sm'])
                        kb.op(D, lambda e: e.max(out=m8r[:], in_=ee[:, 4:20]), reads=['ee'], writes=['m8r'])
                        kb.op(D, lambda e: e.scalar_tensor_tensor(out=sm[:, 18:19], in0=m8r[:, 1:2], scalar=1.0,
                                                                  in1=sm[:, 1:2], op0=ALU.add, op1=ALU.mult),
                              reads=['sm', 'm8r'], writes=['sm'])
                        kb.op(D, lambda e: e.reciprocal(out=sm[:, 19:20], in_=sm[:, 18:19]), reads=['sm'], writes=['sm'])
                        kb.op(D, lambda e: e.scalar_tensor_tensor(out=comb[:], in0=ee[:, 4:20], scalar=sm[:, 19:20],
                                                                  in1=selr[:], op0=ALU.mult, op1=ALU.mult),
                              reads=['ee', 'sm', 'selr'], writes=[ck])
                        kb.op('pe', lambda e: e.transpose(out=tb_[0:16, 0:128], in_=comb[:], identity=ident_f[:]),
                              reads=[ck, 'ident_f'], writes=[tk])
                        c0 = sub * T + s4 * 128
                        kb.op('act', lambda e, c0=c0: e.activation(out=combT[:, c0:c0 + 128], in_=tb_[0:16, 0:128],
                                                                   func=AF.Copy), reads=[tk], writes=['combT'])

                fnorm_p1(0)
                fnorm_p2(0)
                for sub in range(4):
                    if sub + 1 < 4:
                        fnorm_p1(sub + 1)
                    router(sub)
                    if sub + 1 < 4:
                        fnorm_p2(sub + 1)
                stg_list = [(stg[0][:, :], 'stg0'), (stg[1][:, :], 'stg1')] + [
                    (xnf[:, 2 * i:2 * i + 2, :].rearrange("p a b -> p (a b)"), f'stgx{i}') for i in range(4)]

                def moe_piece(ex, p):
                    wb = ex % 2
                    sap, skey = stg_list[(6 * ex + p) % 6]
                    if p < 4:
                        wsrc = I[f'wg{layer}'] if p < 2 else I[f'wu{layer}']
                        half = p % 2
                        src = wsrc[ex][half * 512:(half + 1) * 512, :].rearrange("(c p) n -> p c n", p=128)
                        sview = sap.rearrange("p (c n) -> p c n", c=4)
                        dbuf, dkey = (wgs[wb], f'wgs{wb}') if p < 2 else (wus[wb], f'wus{wb}')
                        dst = dbuf[:, half * 4:(half + 1) * 4, :].rearrange("p c n -> p (c n)")
                    else:
                        fc = p - 4
                        src = I[f'wd{layer}'][ex][fc * 128:(fc + 1) * 128, :]
                        sview = sap
                        dst, dkey = wds[wb][:, fc, :], f'wds{wb}'
                    return sap, skey, sview, src, dst, dkey

                def moe_dma(ex, p):
                    if ex >= 16:
                        return
                    sap, skey, sview, src, dst, dkey = moe_piece(ex, p)
                    kb.dma('sp', sview, src, writes=[skey])

                def moe_cast(ex, p):
                    if ex >= 16:
                        return
                    sap, skey, sview, src, dst, dkey = moe_piece(ex, p)
                    kb.op('act', lambda e: e.activation(out=dst, in_=sap, func=AF.Copy), reads=[skey], writes=[dkey])

                def moe_comb(ex, sub):
                    ss = slice(sub * T, (sub + 1) * T)
                    kb.op('pe', lambda e: e.matmul(ps[0][:], lhsT=sel16[:, ex, :], rhs=combT[:, ss],
                                                   start=True, stop=True), reads=['sel16', 'combT'], writes=['ps0'])

                def moe_au_q(ex, sub, q):
                    wb = ex % 2
                    ss = slice(sub * T, (sub + 1) * T)
                    ab = sub % 2
                    fc, isup = q // 2, q % 2
                    pa, pu = 1 + fc, 3 + fc
                    if not isup:
                        for kc in range(8):
                            kb.op('pe', lambda e, kc=kc: e.matmul(
                                ps[pa][:], lhsT=wgs[wb][:, kc, fc * 128:(fc + 1) * 128], rhs=a32[:, kc, ss],
                                start=(kc == 0), stop=(kc == 7)), reads=[f'wgs{wb}', 'a32'], writes=[f'ps{pa}'])
                        kb.op('act', lambda e: e.activation(out=sa[fc][:], in_=ps[pa][:], func=AF.Silu),
                              reads=[f'ps{pa}'], writes=[f'sa{fc}'])
                    else:
                        for kc in range(8):
                            kb.op('pe', lambda e, kc=kc: e.matmul(
                                ps[pu][:], lhsT=wus[wb][:, kc, fc * 128:(fc + 1) * 128], rhs=a32[:, kc, ss],
                                start=(kc == 0), stop=(kc == 7)), reads=[f'wus{wb}', 'a32'], writes=[f'ps{pu}'])
                        kb.op('dve', lambda e: e.tensor_tensor(out=tt[fc][:], in0=ps[pu][:], in1=sa[fc][:],
                                                               op=ALU.mult),
                              reads=[f'ps{pu}', f'sa{fc}'], writes=[f'tt{fc}'])
                        kb.op('dve', lambda e: e.tensor_tensor(out=actb[ab][:, fc, :], in0=ps[0][:], in1=tt[fc][:],
                                                               op=ALU.mult),
                              reads=['ps0', f'tt{fc}'], writes=[f'actb{ab}'])

                def moe_down_q(ex, sub, q):
                    wb = ex % 2
                    ss = slice(sub * T, (sub + 1) * T)
                    ab = sub % 2
                    for oc in (2 * q, 2 * q + 1):
                        pb = 5 + (oc % 2)
                        for fc in range(2):
                            kb.op('pe', lambda e, fc=fc, oc=oc, pb=pb: e.matmul(
                                ps[pb][:], lhsT=wds[wb][:, fc, oc * 128:(oc + 1) * 128], rhs=actb[ab][:, fc, :],
                                start=(fc == 0), stop=(fc == 1)), reads=[f'wds{wb}', f'actb{ab}'], writes=[f'ps{pb}'])
                        if oc in (3, 7):
                            yb = (oc // 4) % 2
                            kb.op('act', lambda e, pb=pb, yb=yb: e.activation(out=ytmp[yb], in_=ps[pb][:], func=AF.Copy),
                                  reads=[f'ps{pb}'], writes=[f'ytmp{yb}'])
                            kb.op('pool', lambda e, oc=oc, yb=yb: e.tensor_tensor(
                                out=hs[:, oc, ss], in0=hs[:, oc, ss], in1=ytmp[yb], op=ALU.add),
                                reads=[f'ytmp{yb}', f'hs{oc}'], writes=[f'hs{oc}'])
                        else:
                            kb.op('dve', lambda e, oc=oc, pb=pb: e.tensor_tensor(
                                out=hs[:, oc, ss], in0=ps[pb][:], in1=hs[:, oc, ss], op=ALU.add),
                                reads=[f'ps{pb}', f'hs{oc}'], writes=[f'hs{oc}'])

                mitems = [(ex, sub) for ex in range(16) for sub in range(4)]
                kb.op('dve', lambda e: e.tensor_copy(out=sm[:, 41:42], in_=sm[:, 41:42]),
                      reads=['hs', 'sm', 'xnf'],
                      writes=['hs', 'sm', 'xnf'] + [f'hs{oc}' for oc in range(8)] + [f'stgx{i}' for i in range(4)]
                      + [f'xnf_{c}' for c in range(8)])
                for p in range(6):
                    moe_dma(0, p)
                    moe_cast(0, p)
                moe_dma(1, 0)
                moe_dma(1, 1)
                moe_comb(0, 0)
                for q in range(4):
                    moe_au_q(0, 0, q)
                for mi, (ex, sub) in enumerate(mitems):
                    nxt = mitems[mi + 1] if mi + 1 < len(mitems) else None
                    for q in range(4):
                        if nxt is not None:
                            moe_au_q(nxt[0], nxt[1], q)
                            if q == 0:
                                moe_comb(*nxt)
                        moe_down_q(ex, sub, q)
                    if sub == 3:
                        moe_dma(ex + 2, 0)
                        moe_dma(ex + 2, 1)
                    else:
                        moe_cast(ex + 1, 2 * sub)
                        moe_cast(ex + 1, 2 * sub + 1)
                        if sub < 2:
                            moe_dma(ex + 1, 2 * sub + 2)
                            moe_dma(ex + 1, 2 * sub + 3)
                PK = [f'a32p{i}' for i in range(4)] + [f'hsp{i}' for i in range(4)] + [f'ofc{c}' for c in range(8)]
                kb.op('dve', lambda e: e.tensor_copy(out=sm[:, 40:41], in_=sm[:, 40:41]),
                      reads=[f'hs{oc}' for oc in range(8)] + ['sm', 'a32'] + [f'stgx{i}' for i in range(4)],
                      writes=['hs', 'sm', 'xnf', 'a32'] + [f'stgx{i}' for i in range(4)] + PK)
                load_w(wbig, 'wbig', I[f'pg{layer}'], 8, 1024, ceng='act')
                load_w(wpp, 'wpp', I[f'pp{layer}'], 2, 1024, ceng='act')

                def ple_load(sub):
                    for c2 in range(2):
                        kb.dma('sp', pf[:, c2, :], pT_ap[c2 * 128:(c2 + 1) * 128, ts0 + sub * T: ts0 + (sub + 1) * T],
                               writes=['pf', 'ytmp0', 'ytmp1'])

                def ple_cast(sub):
                    kb.op('act', lambda e: e.activation(out=pbf[:].rearrange("p a b -> p (a b)"),
                                                        in_=pf[:].rearrange("p a b -> p (a b)"), func=AF.Copy),
                          reads=['pf'], writes=['pbf'])

                def ple_p1(sub):
                    ss = slice(sub * T, (sub + 1) * T)
                    rmsnorm_p1(lambda c: hs[:, c, ss], f'hsp{sub}', 8, 1024.0, 6)

                def ple_p2(sub):
                    ss = slice(sub * T, (sub + 1) * T)
                    rmsnorm_p2(lambda c: hs[:, c, ss], f'hsp{sub}', 8, lambda c: gains[:, gp, c:c + 1],
                               lambda c: a32[:, c, ss], f'a32p{sub}')

                def ple_oc(sub, oc):
                    ss = slice(sub * T, (sub + 1) * T)
                    pg, pq = (oc % 2), 2 + (oc % 2)
                    fc = oc % 2
                    for kc in range(8):
                        kb.op('pe', lambda e, kc=kc: e.matmul(
                            ps[pg][:], lhsT=wbig[:, kc, oc * 128:(oc + 1) * 128], rhs=a32[:, kc, ss],
                            start=(kc == 0), stop=(kc == 7)), reads=['wbig', f'a32p{sub}'], writes=[f'ps{pg}'])
                    for c2 in range(2):
                        kb.op('pe', lambda e, c2=c2: e.matmul(
                            ps[pq][:], lhsT=wpp[:, c2, oc * 128:(oc + 1) * 128], rhs=pbf[:, c2, :],
                            start=(c2 == 0), stop=(c2 == 1)), reads=['wpp', 'pbf'], writes=[f'ps{pq}'])
                    kb.op('act', lambda e: e.activation(out=sa[fc][:], in_=ps[pg][:], func=AF.Sigmoid),
                          reads=[f'ps{pg}'], writes=[f'sa{fc}'])
                    kb.op('dve', lambda e: e.tensor_tensor(out=tt[fc][:], in0=ps[pq][:], in1=sa[fc][:], op=ALU.mult),
                          reads=[f'ps{pq}', f'sa{fc}'], writes=[f'tt{fc}'])
                    kb.op('pool', lambda e: e.tensor_tensor(out=hs[:, oc, ss], in0=hs[:, oc, ss],
                                                            in1=tt[fc][:], op=ALU.add),
                          reads=[f'hsp{sub}', f'tt{fc}'], writes=[f'hsp{sub}'])

                def ple_final(sub):
                    ss = slice(sub * T, (sub + 1) * T)
                    hk = f'hsp{sub}'
                    for c in range(8):
                        kb.op('act', lambda e, c=c: e.activation(out=sq[:, c, :], in_=hs[:, c, ss], func=AF.Square),
                              reads=[hk], writes=['sq'])
                    for c in range(8):
                        kb.op('pe', lambda e, c=c: e.matmul(ps[6][:], lhsT=ones_bf[:], rhs=sq[:, c, :],
                                                            start=(c == 0), stop=(c == 7)),
                              reads=['sq', 'ones_bf'], writes=['ps6'])
                    kb.op('act', lambda e: e.activation(out=lnv[:], in_=ps[6][:], func=AF.Ln, scale=1.0 / 1024.0,
                                                        bias=eps_t[:, 0:1]), reads=['ps6', 'eps'], writes=['lnv'])
                    kb.op('act', lambda e: e.activation(out=rs_b[:], in_=lnv[:], func=AF.Exp, scale=-0.5),
                          reads=['lnv'], writes=['rs1'])
                    for c in range(8):
                        kb.op('dve', lambda e, c=c: e.scalar_tensor_tensor(
                            out=of[:, c, :], in0=hs[:, c, ss], scalar=gains[:, 6, c:c + 1], in1=rs_b[:],
                            op0=ALU.mult, op1=ALU.mult), reads=[hk, 'rs1', 'gains'], writes=[f'ofc{c}'])
                    for c in range(8):
                        kb.dma('pool', outT[c * 128:(c + 1) * 128, ts0 + sub * T: ts0 + (sub + 1) * T], of[:, c, :],
                               reads=[f'ofc{c}'], writes=['outT'])

                P1EARLY = True
                ple_load(0)
                ple_cast(0)
                ple_p1(0)
                for sub in range(4):
                    more = sub + 1 < 4
                    if sub > 0:
                        ple_load(sub)
                        ple_cast(sub)
                        if not P1EARLY:
                            ple_p1(sub)
                    ple_p2(sub)
                    for oc in range(0, 4):
                        ple_oc(sub, oc)
                    if more and P1EARLY:
                        ple_p1(sub + 1)
                    for oc in range(4, 8):
                        ple_oc(sub, oc)
                    if layer == 1:
                        ple_final(sub)
                    else:
                        ss = slice(sub * T, (sub + 1) * T)
                        for c in range(8):
                            kb.dma('pool', h_dst[c * 128:(c + 1) * 128, ts0 + sub * T:ts0 + (sub + 1) * T], hs[:, c, ss],
                                   reads=[f'hsp{sub}'], writes=['hT'])
                kb.op('dve', lambda e: e.tensor_copy(out=sm[:, 42:43], in_=sm[:, 42:43]),
                      reads=['sm'] + PK, writes=['hs', 'sm', 'xnf', 'a32'] + PK)

        with ExitStack() as es2:
            def sb2(name, shape, dt):
                return es2.enter_context(nc.sbuf_tensor(uname(name), list(shape), dt))
            kmT = sb('kmT', [128, 8, 32], BF16)
            kms = sb2('kms', [128, 8, 32], F32)
            w1 = sb2('w1', [128, 8, 1024], BF16)
            w1s = sb2('w1s', [128, 8, 1024], BF16)
            w2 = sb2('w2', [128, 8, 1024], BF16)
            w3 = sb2('w3', [128, 8, 1024], BF16)
            w3s = sb2('w3s', [128, 8, 1024], BF16)
            xs = [sb2(f'xs{i}', [128, 8, T], F32) for i in range(2)]
            xn = [sb2(f'xn{i}', [128, 8, T], BF16) for i in range(2)]
            cs = [sb2(f'cs{i}', [128, T], F32) for i in range(2)]
            sn = [sb2(f'sn{i}', [128, T], F32) for i in range(2)]
            t1 = [sb2(f't1{i}', [128, T], F32) for i in range(2)]
            t2 = [sb2(f't2{i}', [128, T], F32) for i in range(2)]
            kf = [sb2(f'kf{i}', [128, T], F32) for i in range(2)]
            kbf = sb2('kbf', [128, 8, T], BF16)
            qbf = sb2('qbf', [128, 8, T], BF16)
            vbf = sb2('vbf', [128, 4, 1024], BF16)

            def proj_rope(b, g, wa, was, wk_, wsk_, obuf, okey, dst_dram, dkey, with_kmean, h0, h1):
                gs = slice(g * T, (g + 1) * T)
                for h in range(h0, h1):
                    hb = h % 2
                    p1, p2 = hb, 2 + hb
                    for kc in range(8):
                        kb.op('pe', lambda e, kc=kc: e.matmul(ps[p1][:], lhsT=wa[:, kc, h * 128:(h + 1) * 128],
                                                              rhs=xn[b][:, kc, :], start=(kc == 0), stop=(kc == 7)),
                              reads=[wk_, f'xn{b}'], writes=[f'ps{p1}'])
                    for kc in range(8):
                        kb.op('pe', lambda e, kc=kc: e.matmul(ps[p2][:], lhsT=was[:, kc, h * 128:(h + 1) * 128],
                                                              rhs=xn[b][:, kc, :], start=(kc == 0), stop=(kc == 7)),
                              reads=[wsk_, f'xn{b}'], writes=[f'ps{p2}'])
                    kb.op('dve', lambda e: e.tensor_tensor(out=t1[hb][:], in0=ps[p1][:], in1=cs[b][:], op=ALU.mult),
                          reads=[f'ps{p1}', f'cs{b}'], writes=[f't1{hb}'])
                    kb.op('dve', lambda e: e.tensor_tensor(out=t2[hb][:], in0=ps[p2][:], in1=sn[b][:], op=ALU.mult),
                          reads=[f'ps{p2}', f'sn{b}'], writes=[f't2{hb}'])
                    if with_kmean:
                        kb.op('pool', lambda e: e.tensor_tensor(out=kf[hb][:], in0=t1[hb][:], in1=t2[hb][:], op=ALU.add),
                              reads=[f't1{hb}', f't2{hb}'], writes=[f'kf{hb}'])
                        kb.op('dve', lambda e: e.tensor_reduce(
                            out=kms[:, h, 2 * g:2 * g + 2], in_=kf[hb][:].rearrange("p (a b) -> p a b", a=2),
                            axis=AX.X, op=ALU.add), reads=[f'kf{hb}'], writes=['kms'])
                        kb.op('act', lambda e: e.activation(out=obuf[:, h, :], in_=kf[hb][:], func=AF.Copy),
                              reads=[f'kf{hb}'], writes=[okey])
                    else:
                        kb.op('pool', lambda e: e.tensor_tensor(out=obuf[:, h, :], in0=t1[hb][:], in1=t2[hb][:], op=ALU.add),
                              reads=[f't1{hb}', f't2{hb}'], writes=[okey])
                if h1 == 8:
                    kb.dma('pool', dst_dram[:, gs].rearrange("(h p) t -> p h t", p=128), obuf[:],
                           reads=[okey], writes=[dkey])

            def load_x(b, g):
                gs = slice(g * T, (g + 1) * T)
                for c in range(8):
                    kb.dma('sp', xs[b][:, c, :], I['xT_all'][c * 128:(c + 1) * 128, gs], writes=[f'xs{b}'])
                kb.dma('sp', cs[b][:], I['cosg'][:, gs], writes=[f'cs{b}'])
                kb.dma('sp', sn[b][:], I['sing'][:, gs], writes=[f'sn{b}'])

            def norm_p1(b):
                rmsnorm_p1(lambda c: xs[b][:, c, :], f'xs{b}', 8, 1024.0, 6)

            def norm_p2(b):
                rmsnorm_p2(lambda c: xs[b][:, c, :], f'xs{b}', 8, lambda c: gains[:, 0, c:c + 1],
                           lambda c: xn[b][:, c, :], f'xn{b}')

            load_w(w1, 'w1', I['wk0'], 8, 1024, ceng='act')
            load_x(0, 0)
            load_w(w1s, 'w1s', I['wk0s'], 8, 1024, ceng='act')
            norm_p1(0)
            norm_p2(0)
            load_w(w2, 'w2', I['wv0'], 8, 1024)
            load_w(w3, 'w3', I['wq0'], 8, 1024)
            load_w(w3s, 'w3s', I['wq0s'], 8, 1024)
            for g in range(NGC):
                b = g % 2
                nb_ = 1 - b
                if g + 1 < NGC:
                    load_x(nb_, g + 1)
                proj_rope(b, g, w1, w1s, 'w1', 'w1s', kbf, 'kbf', kT, 'kT', True, 0, 4)
                if g + 1 < NGC:
                    norm_p1(nb_)
                proj_rope(b, g, w1, w1s, 'w1', 'w1s', kbf, 'kbf', kT, 'kT', True, 4, 8)
                for s_ in range(4):
                    for half in range(2):
                        pv = 4 + half
                        for kc in range(8):
                            kb.op('pe', lambda e, kc=kc, s_=s_, half=half, pv=pv: e.matmul(
                                ps[pv][:], lhsT=xn[b][:, kc, s_ * 128:(s_ + 1) * 128],
                                rhs=w2[:, kc, half * 512:(half + 1) * 512], start=(kc == 0), stop=(kc == 7)),
                                reads=['w2', f'xn{b}'], writes=[f'ps{pv}'])
                        kb.op('act', lambda e, s_=s_, half=half, pv=pv: e.activation(
                            out=vbf[:, s_, half * 512:(half + 1) * 512], in_=ps[pv][:], func=AF.Copy),
                            reads=[f'ps{pv}'], writes=[f'vbf{s_}'])
                    kb.dma('pool', vdr[:, :, 4 * g + s_, :].rearrange("h p d -> p h d"),
                           vbf[:, s_, :].rearrange("p (h d) -> p h d", h=8),
                           reads=[f'vbf{s_}'], writes=['vdr'])
                if g + 1 < NGC:
                    norm_p2(nb_)
                proj_rope(b, g, w3, w3s, 'w3', 'w3s', qbf, 'qbf', qT0, 'qsrc', False, 0, 8)
            kb.op('dve', lambda e: e.tensor_scalar(out=kmT[:].rearrange("p a b -> p (a b)"),
                                                   in0=kms[:].rearrange("p a b -> p (a b)"),
                                                   scalar1=1.0 / 256.0, scalar2=None, op0=ALU.mult),
                  reads=['kms'], writes=['kmT'])
        kb.barrier()
        with ExitStack() as es2:
            cbias = es2.enter_context(nc.sbuf_tensor(uname('cbias'), [128, NGC * 128], F32))
            aown = es2.enter_context(nc.sbuf_tensor(uname('aown'), [128, NGC * 128], F32))
            btab = es2.enter_context(nc.sbuf_tensor(uname('btab'), [128, NGC * 128], F32))
            selT = es2.enter_context(nc.sbuf_tensor(uname('selT'), [128, 32, 128], BF16))
            dm0 = es2.enter_context(nc.sbuf_tensor(uname('dm0'), [128, 4, T], BF16))
            kb.dma('sp', cbias[:], I['cbias'], writes=['tabs'])
            kb.dma('sp', aown[:], I['aown'], writes=['tabs'])
            kb.dma('sp', btab[:], I['btab'], writes=['tabs'])
            kb.op('pool', lambda e: e.memset(selT[:], 0.0), writes=['selT'])
            kb.dma('sp', selT[0:32, :, :], I['selT'], writes=['selT'])
            kb.dma('sp', dm0[:], I['dm0'], writes=['dm'])
            attention(0, 128.0 ** -0.5, dm0, es2)
        kb.barrier()
        with ExitStack() as es2:
            rowlocal(0, I['xT_all'], I['wo0'], I['p0T'], es2, 4, hT_all)
        kb.barrier()
        with ExitStack() as es2:
            def sb2(name, shape, dt):
                return es2.enter_context(nc.sbuf_tensor(uname(name), list(shape), dt))
            wdkvc = sb2('wdkvc', [128, 8, 256], BF16)
            wdkvr = sb2('wdkvr', [128, 8, 64], BF16)
            wdkvrs = sb2('wdkvrs', [128, 8, 64], BF16)
            wk = sb2('wk', [128, 2, 1024], BF16)
            wv = sb2('wv', [128, 2, 1024], BF16)
            kvn_t = sb2('kvn_t', [128, 2], F32)
            xs = [sb2(f'xs{i}', [128, 8, T], F32) for i in range(2)]
            xn = [sb2(f'xn{i}', [128, 8, T], BF16) for i in range(2)]
            ckf = sb2('ckf', [128, 2, T], F32)
            ckn = [sb2(f'ckn{i}', [128, 2, T], BF16) for i in range(2)]
            cs = [sb2(f'cs{i}', [128, T], F32) for i in range(2)]
            sn = [sb2(f'sn{i}', [128, T], F32) for i in range(2)]
            t1 = [sb2(f't1{i}', [128, T], F32) for i in range(2)]
            t2 = [sb2(f't2{i}', [128, T], F32) for i in range(2)]
            krb = [sb2(f'krb{i}', [64, T], BF16) for i in range(2)]
            kbf = [sb2(f'kbf{i}', [128, 8, T], BF16) for i in range(2)]
            vbf = [sb2(f'vbf{i}', [128, 4, 1024], BF16) for i in range(2)]
            kb.dma('sp', kvn_t[:], I['kvn'], writes=['qkvn'])
            load_w(wdkvc, 'wdkvc', I['wdkvc'], 8, 256)
            load_w(wdkvr, 'wdkvr', I['wdkvr'], 8, 64)
            load_w(wdkvrs, 'wdkvrs', I['wdkvrs'], 8, 64)
            load_w(wk, 'wk', I['wukvk'], 2, 1024)
            load_w(wv, 'wv', I['wukvv'], 2, 1024)
            def kv_load(b, g):
                gs = slice(g * T, (g + 1) * T)
                for c in range(8):
                    kb.dma('sp', xs[b][:, c, :], hT_all[c * 128:(c + 1) * 128, gs], reads=['hT'], writes=[f'xs{b}'])
                kb.dma('sp', cs[b][:], I['cos64g'][:, gs], writes=[f'cs{b}'])
                kb.dma('sp', sn[b][:], I['sin64g'][:, gs], writes=[f'sn{b}'])

            def kv_p1(b):
                rmsnorm_p1(lambda c: xs[b][:, c, :], f'xs{b}', 8, 1024.0, 6)

            def kv_p2(b):
                rmsnorm_p2(lambda c: xs[b][:, c, :], f'xs{b}', 8, lambda c: gains[:, 1, c:c + 1],
                           lambda c: xn[b][:, c, :], f'xn{b}')

            kv_load(0, 0)
            kv_p1(0)
            kv_p2(0)
            for g in range(NGC):
                b = g % 2
                gs = slice(g * T, (g + 1) * T)
                if g + 1 < NGC:
                    kv_load(1 - b, g + 1)
                for oc in range(2):
                    pb = oc
                    for kc in range(8):
                        kb.op('pe', lambda e, kc=kc, oc=oc, pb=pb: e.matmul(
                            ps[pb][:], lhsT=wdkvc[:, kc, oc * 128:(oc + 1) * 128], rhs=xn[b][:, kc, :],
                            start=(kc == 0), stop=(kc == 7)), reads=['wdkvc', f'xn{b}'], writes=[f'ps{pb}'])
                    kb.op('act', lambda e, oc=oc, pb=pb: e.activation(out=ckf[:, oc, :], in_=ps[pb][:], func=AF.Copy),
                          reads=[f'ps{pb}'], writes=['ckf'])
                rmsnorm(lambda c: ckf[:, c, :], 'ckf', 2, lambda c: kvn_t[:, c:c + 1],
                        lambda c: ckn[b][:, c, :], f'ckn{b}', 256.0, 6)
                if g + 1 < NGC:
                    kv_p1(1 - b)
                for kc in range(8):
                    kb.op('pe', lambda e, kc=kc: e.matmul(ps[2][0:64, :], lhsT=wdkvr[:, kc, :], rhs=xn[b][:, kc, :],
                                                          start=(kc == 0), stop=(kc == 7)),
                          reads=['wdkvr', f'xn{b}'], writes=['ps2'])
                for kc in range(8):
                    kb.op('pe', lambda e, kc=kc: e.matmul(ps[4][0:64, :], lhsT=wdkvrs[:, kc, :], rhs=xn[b][:, kc, :],
                                                          start=(kc == 0), stop=(kc == 7)),
                          reads=['wdkvrs', f'xn{b}'], writes=['ps4'])
                kb.op('dve', lambda e: e.tensor_tensor(out=t1[0][0:64, :], in0=ps[2][0:64, :], in1=cs[b][0:64, :],
                                                       op=ALU.mult), reads=['ps2', f'cs{b}'], writes=['t10'])
                kb.op('dve', lambda e: e.tensor_tensor(out=t2[0][0:64, :], in0=ps[4][0:64, :], in1=sn[b][0:64, :],
                                                       op=ALU.mult), reads=['ps4', f'sn{b}'], writes=['t20'])
                kb.op('pool', lambda e: e.tensor_tensor(out=krb[b][:], in0=t1[0][0:64, :], in1=t2[0][0:64, :],
                                                        op=ALU.add), reads=['t10', 't20'], writes=[f'krb{b}'])
                kb.dma('pool', krT[:, gs], krb[b][:], reads=[f'krb{b}'], writes=['krT'])
                for h in range(8):
                    pb = h % 2
                    for kc in range(2):
                        kb.op('pe', lambda e, kc=kc, h=h, pb=pb: e.matmul(
                            ps[pb][:], lhsT=wk[:, kc, h * 128:(h + 1) * 128], rhs=ckn[b][:, kc, :],
                            start=(kc == 0), stop=(kc == 1)), reads=['wk', f'ckn{b}'], writes=[f'ps{pb}'])
                    kb.op('act', lambda e, h=h, pb=pb: e.activation(out=kbf[b][:, h, :], in_=ps[pb][:], func=AF.Copy),
                          reads=[f'ps{pb}'], writes=[f'kbf{b}'])
                kb.dma('pool', kT[:, gs].rearrange("(h p) t -> p h t", p=128), kbf[b][:],
                       reads=[f'kbf{b}'], writes=['kT'])
                if g + 1 < NGC:
                    kv_p2(1 - b)
                for s_ in range(4):
                    for half in range(2):
                        pv = 2 + 2 * half
                        pv = 3 if half == 0 else 5
                        for kc in range(2):
                            kb.op('pe', lambda e, kc=kc, s_=s_, half=half, pv=pv: e.matmul(
                                ps[pv][:], lhsT=ckn[b][:, kc, s_ * 128:(s_ + 1) * 128],
                                rhs=wv[:, kc, half * 512:(half + 1) * 512], start=(kc == 0), stop=(kc == 1)),
                                reads=['wv', f'ckn{b}'], writes=[f'ps{pv}'])
                        kb.op('dve', lambda e, s_=s_, half=half, pv=pv: e.tensor_copy(
                            out=vbf[b][:, s_, half * 512:(half + 1) * 512], in_=ps[pv][:]),
                            reads=[f'ps{pv}'], writes=[f'vbf{b}'])
                    kb.dma('pool', vdr[:, :, 4 * g + s_, :].rearrange("h p d -> p h d"),
                           vbf[b][:, s_, :].rearrange("p (h d) -> p h d", h=8),
                           reads=[f'vbf{b}'], writes=['vdr'])
        kb.barrier()
        with ExitStack() as es2:
            def sb2(name, shape, dt):
                return es2.enter_context(nc.sbuf_tensor(uname(name), list(shape), dt))
            wdq = sb2('wdq', [128, 8, 384], BF16)
            wuqn = sb2('wuqn', [128, 3, 1024], BF16)
            wuqr = sb2('wuqr', [128, 3, 512], BF16)
            wuqrs = sb2('wuqrs', [128, 3, 512], BF16)
            qn_t = sb2('qn_t', [128, 3], F32)
            sel2 = sb2('sel2', [128, 2], F32)
            xs = [sb2(f'xs{i}', [128, 8, T], F32) for i in range(2)]
            xs2 = sb2('xs2', [128, 8, T], F32)
            xn = [sb2(f'xn{i}', [128, 8, T], BF16) for i in range(2)]
            cqf = sb2('cqf', [128, 3, T], F32)
            cqn = sb2('cqn', [128, 3, T], BF16)
            cs = [sb2(f'cs{i}', [128, T], F32) for i in range(2)]
            sn = [sb2(f'sn{i}', [128, T], F32) for i in range(2)]
            t1 = [sb2(f't1{i}', [128, T], F32) for i in range(2)]
            t2 = [sb2(f't2{i}', [128, T], F32) for i in range(2)]
            qnb = [sb2(f'qnb{i}', [128, 8, T], BF16) for i in range(2)]
            qrb = [sb2(f'qrb{i}', [128, 4, T], BF16) for i in range(2)]
            kb.dma('sp', qn_t[:], I['qn'], writes=['qkvn'])
            kb.dma('sp', sel2[:], I['sel2'], writes=['sel2'])
            load_w(wdq, 'wdq', I['wdq'], 8, 384)
            load_w(wuqn, 'wuqn', I['wuqn'], 3, 1024)
            load_w(wuqr, 'wuqr', I['wuqr'], 3, 512)
            load_w(wuqrs, 'wuqrs', I['wuqrs'], 3, 512)
            def q_load(b, j):
                js = slice(j * T, (j + 1) * T)
                ga = slice((2 * j) * T, (2 * j + 1) * T)
                gb_ = slice((2 * j + 1) * T, (2 * j + 2) * T)
                for c in range(8):
                    kb.dma('sp', xs[b][:, c, :], hT_all[c * 128:(c + 1) * 128, ga], reads=['hT'], writes=[f'xs{b}'])
                    kb.dma('sp', xs2[:, c, :], hT_all[c * 128:(c + 1) * 128, gb_], reads=['hT'], writes=['xs2'])
                kb.dma('sp', cs[b][:], I['cos64'][:, js], writes=[f'cs{b}'])
                kb.dma('sp', sn[b][:], I['sin64'][:, js], writes=[f'sn{b}'])

            def q_select(b, j):
                js = slice(j * T, (j + 1) * T)
                for c in range(8):
                    kb.op('dve', lambda e, c=c: e.tensor_scalar(out=xs[b][:, c, :], in0=xs[b][:, c, :], scalar1=sel2[:, 0:1],
                                                                scalar2=None, op0=ALU.mult),
                          reads=[f'xs{b}', 'sel2'], writes=[f'xs{b}'])
                    kb.op('dve', lambda e, c=c: e.scalar_tensor_tensor(
                        out=xs[b][:, c, :], in0=xs2[:, c, :], scalar=sel2[:, 1:2], in1=xs[b][:, c, :],
                        op0=ALU.mult, op1=ALU.add), reads=[f'xs{b}', 'xs2', 'sel2'], writes=[f'xs{b}'])
                for c in range(8):
                    kb.dma('sp', hT_loc[c * 128:(c + 1) * 128, js], xs[b][:, c, :], reads=[f'xs{b}'], writes=['hTl'])

            def q_p1(b):
                rmsnorm_p1(lambda c: xs[b][:, c, :], f'xs{b}', 8, 1024.0, 6)

            def q_p2(b):
                rmsnorm_p2(lambda c: xs[b][:, c, :], f'xs{b}', 8, lambda c: gains[:, 1, c:c + 1],
                           lambda c: xn[b][:, c, :], f'xn{b}')

            q_load(0, 0)
            q_select(0, 0)
            q_p1(0)
            q_p2(0)
            for j in range(NJ):
                b = j % 2
                js = slice(j * T, (j + 1) * T)
                if j + 1 < NJ:
                    q_load(1 - b, j + 1)
                for oc in range(3):
                    pb = oc % 2
                    for kc in range(8):
                        kb.op('pe', lambda e, kc=kc, oc=oc, pb=pb: e.matmul(
                            ps[pb][:], lhsT=wdq[:, kc, oc * 128:(oc + 1) * 128], rhs=xn[b][:, kc, :],
                            start=(kc == 0), stop=(kc == 7)), reads=['wdq', f'xn{b}'], writes=[f'ps{pb}'])
                    kb.op('act', lambda e, oc=oc, pb=pb: e.activation(out=cqf[:, oc, :], in_=ps[pb][:], func=AF.Copy),
                          reads=[f'ps{pb}'], writes=['cqf'])
                rmsnorm(lambda c: cqf[:, c, :], 'cqf', 3, lambda c: qn_t[:, c:c + 1],
                        lambda c: cqn[:, c, :], 'cqn', 384.0, 6)
                if j + 1 < NJ:
                    q_select(1 - b, j + 1)
                    q_p1(1 - b)
                for h in range(8):
                    pb = h % 2
                    for kc in range(3):
                        kb.op('pe', lambda e, kc=kc, h=h, pb=pb: e.matmul(
                            ps[pb][:], lhsT=wuqn[:, kc, h * 128:(h + 1) * 128], rhs=cqn[:, kc, :],
                            start=(kc == 0), stop=(kc == 2)), reads=['wuqn', 'cqn'], writes=[f'ps{pb}'])
                    kb.op('act', lambda e, h=h, pb=pb: e.activation(out=qnb[b][:, h, :], in_=ps[pb][:], func=AF.Copy),
                          reads=[f'ps{pb}'], writes=[f'qnb{b}'])
                kb.dma('pool', qnT[:, js].rearrange("(h p) t -> p h t", p=128), qnb[b][:],
                       reads=[f'qnb{b}'], writes=['qsrc'])
                if j + 1 < NJ:
                    q_p2(1 - b)
                for hp in range(4):
                    pb = hp % 2
                    p1, p2 = 2 + pb, 4 + pb
                    for kc in range(3):
                        kb.op('pe', lambda e, kc=kc, hp=hp, p1=p1: e.matmul(
                            ps[p1][:], lhsT=wuqr[:, kc, hp * 128:(hp + 1) * 128], rhs=cqn[:, kc, :],
                            start=(kc == 0), stop=(kc == 2)), reads=['wuqr', 'cqn'], writes=[f'ps{p1}'])
                    for kc in range(3):
                        kb.op('pe', lambda e, kc=kc, hp=hp, p2=p2: e.matmul(
                            ps[p2][:], lhsT=wuqrs[:, kc, hp * 128:(hp + 1) * 128], rhs=cqn[:, kc, :],
                            start=(kc == 0), stop=(kc == 2)), reads=['wuqrs', 'cqn'], writes=[f'ps{p2}'])
                    kb.op('dve', lambda e, pb=pb, p1=p1: e.tensor_tensor(out=t1[pb][:], in0=ps[p1][:], in1=cs[b][:],
                                                                         op=ALU.mult),
                          reads=[f'ps{p1}', f'cs{b}'], writes=[f't1{pb}'])
                    kb.op('dve', lambda e, pb=pb, p2=p2: e.tensor_tensor(out=t2[pb][:], in0=ps[p2][:], in1=sn[b][:],
                                                                         op=ALU.mult),
                          reads=[f'ps{p2}', f'sn{b}'], writes=[f't2{pb}'])
                    kb.op('pool', lambda e, pb=pb, hp=hp: e.tensor_tensor(out=qrb[b][:, hp, :], in0=t1[pb][:],
                                                                          in1=t2[pb][:], op=ALU.add),
                          reads=[f't1{pb}', f't2{pb}'], writes=[f'qrb{b}'])
                kb.dma('pool', qrT[:, js].rearrange("(h p) t -> p h t", p=128), qrb[b][:],
                       reads=[f'qrb{b}'], writes=['qrT'])
        kb.barrier()
        with ExitStack() as es2:
            dm1 = es2.enter_context(nc.sbuf_tensor(uname('dm1'), [128, 8, T], BF16))
            kb.dma('sp', dm1[:], I['dm1'], writes=['dm'])
            attention(1, 192.0 ** -0.5, dm1, es2)
        kb.barrier()
        with ExitStack() as es2:
            rowlocal(1, hT_loc, I['wo1'], I['p1T'], es2, 2, None)
        kb.barrier()
    return nc


def _rope_tables(pos, dim):
    half = dim // 2
    inv = (np.float32(10000.0) ** (-np.arange(half, dtype=np.float32) / np.float32(half))).astype(np.float32)
    ang = (pos.astype(np.float32)[:, None] * inv[None, :]).astype(np.float32)
    cos, sin = np.cos(ang).astype(np.float32), np.sin(ang).astype(np.float32)
    cosT = np.concatenate([cos, cos], axis=1).T
    sinT = np.concatenate([-sin, sin], axis=1).T
    return np.ascontiguousarray(cosT), np.ascontiguousarray(sinT)


def _swap_halves(w, n_heads, dim):
    k = w.shape[0]
    w4 = w.reshape(k, n_heads, 2, dim // 2)
    return np.ascontiguousarray(w4[:, :, ::-1, :].reshape(k, n_heads * dim))


def _consts():
    c = {}
    c['ones_bf'] = np.ones((128, 128), NPBF)
    c['ident_bf'] = np.eye(128, dtype=np.float32).astype(NPBF)
    c['ident_f'] = np.eye(128, dtype=np.float32)
    s16 = np.zeros((16, 16, 128), np.float32)
    for n in range(16):
        s16[n, n, :] = 1.0
    c['sel16'] = s16.astype(NPBF)
    s32 = np.zeros((32, 32, 128), np.float32)
    for n in range(32):
        s32[n, n, :] = 1.0
    c['selT'] = s32.astype(NPBF)
    return c


def _moba_tables():
    t = {}
    cb = np.zeros((128, NGC, 4, 32), np.float32)
    ao = np.zeros((128, NGC, 4, 32), np.float32)
    bt = np.zeros((128, NGC, 4, 32), np.float32)
    n = np.arange(32)
    for g in range(NGC):
        for s in range(4):
            own = g * 2 + s // 2
            cb[:, g, s, :] = np.where(n < own, 0.0, NEG)[None, :]
            ao[:, g, s, :] = (n == own).astype(np.float32)[None, :]
            bt[:, g, s, :] = np.where(n > own, -BIG, 0.0)[None, :]
    t['cbias'] = cb.reshape(128, -1)
    t['aown'] = ao.reshape(128, -1)
    t['btab'] = bt.reshape(128, -1)
    p = np.arange(128)[:, None]
    c = np.arange(T)[None, :]
    dm0 = np.zeros((128, 4, T), np.float32)
    for i in range(4):
        k = i * 128 + p
        dm0[:, i, :] = np.where(((k // 256) == (c // 256)) & (k > c), -BIG, 0.0)
    t['dm0'] = dm0.astype(NPBF)
    return t


def _core_tables(hc):
    t = {}
    p = np.arange(128)[:, None]
    c = np.arange(T)[None, :]
    dm1 = np.zeros((128, 8, T), np.float32)
    for i in range(8):
        chunk_b = i >= 4
        k = (i % 4) * 128 + p
        own = (chunk_b == (hc == 1))
        if own:
            dm1[:, i, :] = np.where(k > c, -BIG, 0.0)
        else:
            dm1[:, i, :] = -BIG if (hc == 0 and chunk_b) else 0.0
    t['dm1'] = dm1.astype(NPBF)
    s2 = np.zeros((128, 2), np.float32)
    s2[:, hc] = 1.0
    t['sel2'] = s2
    return t


def _prep(inputs):
    f = lambda a: np.ascontiguousarray(np.asarray(a, dtype=np.float32))
    x = f(inputs['x'])
    p = f(inputs['p'])
    common = _consts()
    common.update(_moba_tables())
    g = np.stack([f(inputs['attn_norm'])[0], f(inputs['attn_norm'])[1], f(inputs['ffn_norm'])[0],
                  f(inputs['ffn_norm'])[1], f(inputs['ple_norm'])[0], f(inputs['ple_norm'])[1],
                  f(inputs['final_norm'])], axis=0)
    common['gains'] = np.ascontiguousarray(g.reshape(7, 8, 128).transpose(2, 0, 1))
    wqkv = f(inputs['moba_wqkv'])[0]
    wq, wk, wv = wqkv[:, 0:1024], wqkv[:, 1024:2048], wqkv[:, 2048:3072]
    common['wq0'] = np.ascontiguousarray(wq)
    common['wq0s'] = _swap_halves(wq, 8, 128)
    common['wk0'] = np.ascontiguousarray(wk)
    common['wk0s'] = _swap_halves(wk, 8, 128)
    common['wv0'] = np.ascontiguousarray(wv)
    common['wo0'] = f(inputs['moba_wo'])[0]
    common['wdq'] = f(inputs['mla_wdq'])[0]
    wuq = f(inputs['mla_wuq'])[0].reshape(384, 8, 192)
    common['wuqn'] = np.ascontiguousarray(wuq[:, :, :128].reshape(384, 1024))
    wr_ = np.ascontiguousarray(wuq[:, :, 128:].reshape(384, 512))
    common['wuqr'] = wr_
    common['wuqrs'] = _swap_halves(wr_, 8, 64)
    wdkv = f(inputs['mla_wdkv'])[0]
    common['wdkvc'] = np.ascontiguousarray(wdkv[:, :256])
    common['wdkvr'] = np.ascontiguousarray(wdkv[:, 256:])
    common['wdkvrs'] = _swap_halves(np.ascontiguousarray(wdkv[:, 256:]), 1, 64)
    common['qn'] = np.ascontiguousarray(f(inputs['mla_qnorm'])[0].reshape(3, 128).T)
    common['kvn'] = np.ascontiguousarray(f(inputs['mla_kvnorm'])[0].reshape(2, 128).T)
    wukv = f(inputs['mla_wukv'])[0].reshape(256, 8, 256)
    common['wukvk'] = np.ascontiguousarray(wukv[:, :, :128].reshape(256, 1024))
    common['wukvv'] = np.ascontiguousarray(wukv[:, :, 128:].reshape(256, 1024))
    common['wo1'] = f(inputs['mla_wo'])[0]
    for li in range(2):
        common[f'wr{li}'] = np.ascontiguousarray(
            np.concatenate([f(inputs['moe_wgroup'])[li], f(inputs['moe_wexpert'])[li]], axis=1))
        common[f'wg{li}'] = f(inputs['moe_wgate'])[li]
        common[f'wu{li}'] = f(inputs['moe_wup'])[li]
        common[f'wd{li}'] = f(inputs['moe_wdown'])[li]
        common[f'pg{li}'] = f(inputs['ple_gate'])[li]
        common[f'pp{li}'] = f(inputs['ple_proj'])[li]
    cosg, sing = _rope_tables(np.arange(NG), 128)
    common['cosg'], common['sing'] = cosg, sing
    c64g, s64g = _rope_tables(np.arange(NG), 64)
    common['cos64g'] = np.ascontiguousarray(np.concatenate([c64g, c64g], axis=0))
    common['sin64g'] = np.ascontiguousarray(np.concatenate([s64g, s64g], axis=0))
    tabs = [_core_tables(0), _core_tables(1)]
    locs = []
    per_core = []
    xT = [np.ascontiguousarray(x[b].T) for b in range(4)]
    p0T = [np.ascontiguousarray(p[0, b].T) for b in range(4)]
    for c in range(8):
        b, hc = c // 2, c % 2
        loc = np.concatenate([np.arange((2 * j + hc) * T, (2 * j + hc + 1) * T) for j in range(NJ)])
        locs.append(loc)
        d = dict(common)
        d.update(tabs[hc])
        d['xT_all'] = xT[b]
        d['p0T'] = p0T[b]
        d['p1T'] = np.ascontiguousarray(p[1, b][loc].T)
        c64, s64 = _rope_tables(loc, 64)
        d['cos64'] = np.ascontiguousarray(np.concatenate([c64, c64], axis=0))
        d['sin64'] = np.ascontiguousarray(np.concatenate([s64, s64], axis=0))
        per_core.append(d)
    return per_core, locs


_NC_CACHE = {}


def _get_nc(mode):
    if mode not in _NC_CACHE:
        _NC_CACHE[mode] = build(mode)
    return _NC_CACHE[mode]


def kernel(**inputs):
    per_core, locs = _prep(inputs)
    nc = _get_nc('fused')
    res = run_bass_kernel_spmd(nc, per_core, core_ids=list(range(8)))
    out = np.empty((4, NG, 1024), np.float32)
    for c in range(8):
        out[c // 2][locs[c]] = np.asarray(res.results[c]['outT'], dtype=np.float32).T
    return out
```

```python
import numpy as np
import ml_dtypes
from contextlib import ExitStack
import concourse.bass as bass
import concourse.mybir as mybir
from concourse.bass_utils import run_bass_kernel_spmd

F32, BF16 = mybir.dt.float32, mybir.dt.bfloat16
AF = mybir.ActivationFunctionType
ALU = mybir.AluOpType
AX = mybir.AxisListType
BIG = 30000.0
NEG = -1.0e30
T = 512
NL = 4096
NG = 8192
NJ = 8
NGC = 16
RMS_EPS = 1e-6
NPBF = ml_dtypes.bfloat16


class KB:
    def __init__(self, nc, es):
        self.nc = nc
        self.h = {'pe': nc.tensor, 'act': nc.scalar, 'dve': nc.vector, 'pool': nc.gpsimd, 'sp': nc.sync}
        self.csem = {e: es.enter_context(nc.semaphore("c_" + e)) for e in ('pe', 'act', 'dve', 'pool')}
        self.cnt = {e: 0 for e in self.csem}
        NQ = 8
        self.dsem = {q: [es.enter_context(nc.semaphore(f"d_{q}{i}")) for i in range(NQ)] for q in ('sp', 'pool')}
        self.dval = {q: [0] * NQ for q in self.dsem}
        self.dnext = {q: 0 for q in self.dsem}
        self.waited = {e: {} for e in self.h}
        self.lastw = {}
        self.readers = {}

    def _wait(self, eng, tok):
        semkey, sem, val, src = tok
        if self.waited[eng].get(semkey, 0) >= val:
            return
        self.h[eng].wait_ge(sem, val)
        self.waited[eng][semkey] = val

    def _deps(self, eng, reads, writes):
        for k in reads:
            t = self.lastw.get(k)
            if t is not None and not (t[3] == 'pe' and eng == 'pe'):
                self._wait(eng, t)
        for k in writes:
            t = self.lastw.get(k)
            if t is not None and not (t[3] == 'pe' and eng == 'pe'):
                self._wait(eng, t)
            for t in self.readers.get(k, ()):
                if t[3] != eng:
                    self._wait(eng, t)

    def _record(self, tok, reads, writes):
        for k in writes:
            self.lastw[k] = tok
            self.readers[k] = []
        for k in reads:
            self.readers.setdefault(k, []).append(tok)

    def op(self, eng, fn, reads=(), writes=()):
        self._deps(eng, reads, writes)
        inst = fn(self.h[eng])
        self.cnt[eng] += 1
        inst.then_inc(self.csem[eng], 1)
        tok = ('c' + eng, self.csem[eng], self.cnt[eng], eng)
        self._record(tok, reads, writes)
        return tok

    def dma(self, q, out, in_, reads=(), writes=()):
        slot = self.dnext[q]
        self.dnext[q] = (slot + 1) % len(self.dsem[q])
        sem = self.dsem[q][slot]
        prev = ('d' + q + str(slot), sem, self.dval[q][slot], 'dma')
        if prev[2] > 0:
            self._wait(q, prev)
        self._deps(q, reads, writes)
        self.h[q].dma_start(out=out, in_=in_).then_inc(sem, 16)
        self.dval[q][slot] += 16
        tok = ('d' + q + str(slot), sem, self.dval[q][slot], 'dma')
        self._record(tok, reads, writes)
        return tok

    def barrier(self):
        toks = []
        for e in self.csem:
            if self.cnt[e] > 0:
                toks.append(('c' + e, self.csem[e], self.cnt[e], e))
        for q in self.dsem:
            for i, s in enumerate(self.dsem[q]):
                if self.dval[q][i] > 0:
                    toks.append(('d' + q + str(i), s, self.dval[q][i], 'dma'))
        for e in self.h:
            for t in toks:
                if t[3] == e:
                    continue
                self._wait(e, t)
        self.lastw = {}
        self.readers = {}


def build(mode='fused'):
    nc = bass.Bass("TRN2", target_bir_lowering=False)

    def din(name, shape, dt=F32):
        return nc.dram_tensor(name, list(shape), dt, kind="ExternalInput").ap()

    def dout(name, shape, dt=F32):
        return nc.dram_tensor(name, list(shape), dt, kind="ExternalOutput").ap()

    def dint(name, shape, dt=F32):
        return nc.dram_tensor(name, list(shape), dt, kind="Internal").ap()

    I = {}
    I['gains'] = din('gains', [128, 7, 8])
    I['ones_bf'] = din('ones_bf', [128, 128], BF16)
    I['ident_bf'] = din('ident_bf', [128, 128], BF16)
    I['ident_f'] = din('ident_f', [128, 128])
    I['sel16'] = din('sel16', [16, 16, 128], BF16)
    I['sel2'] = din('sel2', [128, 2])
    I['xT_all'] = din('xT_all', [1024, NG])
    I['p0T'] = din('p0T', [256, NG])
    I['p1T'] = din('p1T', [256, NL])
    I['cosg'] = din('cosg', [128, NG])
    I['sing'] = din('sing', [128, NG])
    I['cos64g'] = din('cos64g', [128, NG])
    I['sin64g'] = din('sin64g', [128, NG])
    I['cos64'] = din('cos64', [128, NL])
    I['sin64'] = din('sin64', [128, NL])
    for n in ('wq0', 'wq0s', 'wk0', 'wk0s', 'wv0', 'wo0', 'wo1'):
        I[n] = din(n, [1024, 1024])
    I['cbias'] = din('cbias', [128, NGC * 4 * 32])
    I['aown'] = din('aown', [128, NGC * 4 * 32])
    I['btab'] = din('btab', [128, NGC * 4 * 32])
    I['selT'] = din('selT', [32, 32, 128], BF16)
    I['dm0'] = din('dm0', [128, 4, T], BF16)
    I['dm1'] = din('dm1', [128, 8, T], BF16)
    I['wdq'] = din('wdq', [1024, 384])
    I['wuqn'] = din('wuqn', [384, 1024])
    I['wuqr'] = din('wuqr', [384, 512])
    I['wuqrs'] = din('wuqrs', [384, 512])
    I['wdkvc'] = din('wdkvc', [1024, 256])
    I['wdkvr'] = din('wdkvr', [1024, 64])
    I['wdkvrs'] = din('wdkvrs', [1024, 64])
    I['qn'] = din('qn', [128, 3])
    I['kvn'] = din('kvn', [128, 2])
    I['wukvk'] = din('wukvk', [256, 1024])
    I['wukvv'] = din('wukvv', [256, 1024])
    for li in (0, 1):
        I[f'wr{li}'] = din(f'wr{li}', [1024, 20])
        I[f'wg{li}'] = din(f'wg{li}', [16, 1024, 256])
        I[f'wu{li}'] = din(f'wu{li}', [16, 1024, 256])
        I[f'wd{li}'] = din(f'wd{li}', [16, 256, 1024])
        I[f'pg{li}'] = din(f'pg{li}', [1024, 1024])
        I[f'pp{li}'] = din(f'pp{li}', [256, 1024])

    hT_all = dint('hT_all', [1024, NG], F32)
    hT_loc = dint('hT_loc', [1024, NL], F32)
    qnT = dint('qnT', [1024, NL], BF16)
    qrT = dint('qrT', [512, NL], BF16)
    kT = dint('kT', [1024, NG], BF16)
    krT = dint('krT', [64, NG], BF16)
    vdr = dint('vdr', [8, 128, 64, 128], BF16)
    qT0 = dint('qT0', [1024, NG], BF16)
    oT = dint('oT', [1024, NG], BF16)
    outT = dout('outT', [1024, NL], F32)

    es = ExitStack()
    with es:
        kb = KB(nc, es)

        uid = [0]

        def uname(name):
            uid[0] += 1
            return f"s{uid[0]}_{name}"

        def sb(name, shape, dt):
            return es.enter_context(nc.sbuf_tensor(uname(name), list(shape), dt))

        gains = sb('gains', [128, 7, 8], F32)
        ones_bf = sb('ones_bf', [128, 128], BF16)
        ident_bf = sb('ident_bf', [128, 128], BF16)
        ident_f = sb('ident_f', [128, 128], F32)
        sel16 = sb('sel16', [16, 16, 128], BF16)
        kb.dma('sp', gains[:], I['gains'], writes=['gains'])
        kb.dma('sp', ones_bf[:], I['ones_bf'], writes=['ones_bf'])
        kb.dma('sp', ident_bf[:], I['ident_bf'], writes=['ident_bf'])
        kb.dma('sp', ident_f[:], I['ident_f'], writes=['ident_f'])
        kb.dma('sp', sel16[:], I['sel16'], writes=['sel16'])
        CONSTK = ['gains', 'ones_bf', 'ident_bf', 'ident_f', 'sel16']

        ps = [es.enter_context(nc.psum_tensor(f"ps{i}", [128, T], F32)) for i in range(7)]
        psb = es.enter_context(nc.psum_tensor("psb", [128, 2 * T], BF16))

        stg = [sb(f'stg{i}', [128, 1024], F32) for i in range(2)]
        stg_i = [0]

        def load_w(dst, dkey, src, kc_n, N, rows=128, ceng='pool'):
            for kc in range(kc_n):
                for n0 in range(0, N, 1024):
                    nn = min(1024, N - n0)
                    i = stg_i[0]
                    stg_i[0] = (i + 1) % 2
                    kb.dma('sp', stg[i][:rows, :nn], src[kc * rows:(kc + 1) * rows, n0:n0 + nn],
                           writes=[f'stg{i}'])
                    if ceng == 'act':
                        kb.op('act', lambda e, i=i, kc=kc, n0=n0, nn=nn: e.activation(
                            out=dst[:rows, kc, n0:n0 + nn], in_=stg[i][:rows, :nn], func=AF.Copy),
                            reads=[f'stg{i}'], writes=[dkey])
                    else:
                        kb.op('pool', lambda e, i=i, kc=kc, n0=n0, nn=nn: e.tensor_copy(
                            out=dst[:rows, kc, n0:n0 + nn], in_=stg[i][:rows, :nn]),
                            reads=[f'stg{i}'], writes=[dkey])

        sq = sb('sq', [128, 8, T], BF16)
        rs = sb('rs', [128, T], F32)
        lnv = sb('lnv', [128, T], F32)

        rs_b = sb('rs_b', [128, T], F32)
        rsl = [rs, rs_b]

        def rmsnorm_p1(src_fn, skey, nch, Dn, psbank, ri=0):
            for c in range(nch):
                kb.op('act', lambda e, c=c: e.activation(out=sq[:, c, :], in_=src_fn(c), func=AF.Square),
                      reads=[skey], writes=['sq'])
            for c in range(nch):
                kb.op('pe', lambda e, c=c: e.matmul(ps[psbank][:], lhsT=ones_bf[:], rhs=sq[:, c, :],
                                                    start=(c == 0), stop=(c == nch - 1)),
                      reads=['sq', 'ones_bf'], writes=[f'ps{psbank}'])
            kb.op('act', lambda e: e.activation(out=lnv[:], in_=ps[psbank][:], func=AF.Ln,
                                                scale=1.0 / Dn, bias=eps_t[:, 0:1]),
                  reads=[f'ps{psbank}', 'eps'], writes=['lnv'])
            kb.op('act', lambda e: e.activation(out=rsl[ri][:], in_=lnv[:], func=AF.Exp, scale=-0.5),
                  reads=['lnv'], writes=[f'rs{ri}'])

        def rmsnorm_p2(src_fn, skey, nch, gcol_fn, dst_fn, dkey, ri=0, dst_f_fn=None, dfkey=None):
            for c in range(nch):
                if dst_f_fn is not None:
                    kb.op('dve', lambda e, c=c: e.scalar_tensor_tensor(
                        out=dst_f_fn(c), in0=src_fn(c), scalar=gcol_fn(c), in1=rsl[ri][:],
                        op0=ALU.mult, op1=ALU.mult), reads=[skey, f'rs{ri}', 'gains', 'qkvn'],
                        writes=[dfkey, f'{dfkey}_{c}'])
                    kb.op('pool', lambda e, c=c: e.tensor_copy(out=dst_fn(c), in_=dst_f_fn(c)),
                          reads=[f'{dfkey}_{c}'], writes=[dkey])
                else:
                    kb.op('dve', lambda e, c=c: e.scalar_tensor_tensor(
                        out=dst_fn(c), in0=src_fn(c), scalar=gcol_fn(c), in1=rsl[ri][:],
                        op0=ALU.mult, op1=ALU.mult), reads=[skey, f'rs{ri}', 'gains', 'qkvn'], writes=[dkey])

        def rmsnorm(src_fn, skey, nch, gcol_fn, dst_fn, dkey, Dn, psbank, dst_f_fn=None, dfkey=None):
            rmsnorm_p1(src_fn, skey, nch, Dn, psbank, 0)
            rmsnorm_p2(src_fn, skey, nch, gcol_fn, dst_fn, dkey, 0, dst_f_fn, dfkey)

        eps_t = sb('eps_t', [128, 1], F32)
        kb.op('dve', lambda e: e.memset(eps_t[:], RMS_EPS), writes=['eps'])

        def attention(layer, scale, dm_sb, es2):
            def sb2(name, shape, dt):
                return es2.enter_context(nc.sbuf_tensor(uname(name), list(shape), dt))
            mla = layer == 1
            NQT = NJ if mla else NGC
            kTh = [sb2(f'kTh{i}', [128, NG], BF16) for i in range(2)]
            vh = [sb2(f'vh{i}', [128, 64, 128], BF16) for i in range(2)]
            qTh = [sb2(f'qTh{i}', [128, NQT * T], BF16) for i in range(2)]
            if mla:
                krs = sb2('krs', [128, NG], BF16)
                qrh = [sb2(f'qrh{i}', [128, NL], BF16) for i in range(2)]
                kb.op('pool', lambda e: e.memset(krs[64:128, :], 0.0), writes=['krs'])
                for i in range(2):
                    kb.op('pool', lambda e, i=i: e.memset(qrh[i][64:128, :], 0.0), writes=[f'qrh{i}'])
                kb.dma('sp', krs[0:64, :], krT, reads=['krT'], writes=['krs'])
            else:
                fm = sb2('fm', [128, 4, 32], BF16)
                gm = sb2('gm', [128, 4, 32], F32)
                selm = sb2('selm', [128, 4, 32], F32)
                m8 = sb2('m8', [128, 4, 8], F32)
                fmT = [sb2(f'fmT{i}', [128, T], BF16) for i in range(2)]
                for i in range(2):
                    kb.op('pool', lambda e, i=i: e.memset(fmT[i][:], 0.0), writes=[f'fmT{i}'])
            pT = [sb2(f'pT{i}', [128, T], BF16) for i in range(2)]
            rinv = sb2('rinv', [128, T], F32)
            ost = [sb2(f'ost{i}', [128, T], BF16) for i in range(2)]

            def load_head(h):
                hb = h % 2
                for part in range(4):
                    kb.dma('sp', kTh[hb][:, part * 2048:(part + 1) * 2048],
                           kT[h * 128:(h + 1) * 128, part * 2048:(part + 1) * 2048],
                           reads=['kT'], writes=[f'kTh{hb}'])
                for part in range(4):
                    kb.dma('sp', vh[hb][:, part * 16:(part + 1) * 16, :], vdr[h, :, part * 16:(part + 1) * 16, :],
                           reads=['vdr'], writes=[f'vh{hb}'])
                qsrc = qnT if mla else qT0
                kb.dma('sp', qTh[hb][:], qsrc[h * 128:(h + 1) * 128, :], reads=['qsrc'], writes=[f'qTh{hb}'])
                if mla:
                    kb.dma('sp', qrh[hb][0:64, :], qrT[h * 64:(h + 1) * 64, :], reads=['qrT'], writes=[f'qrh{hb}'])

            def geom(j):
                nkt = 4 * (2 * j + 2) if mla else 4 * (j + 1)
                dstart = 8 * j if mla else 4 * j
                return nkt, dstart

            def qk(h, j, ob, kt):
                hb = h % 2
                nkt, dstart = geom(j)
                qs = slice(j * T, (j + 1) * T)
                sbk = kt % 2
                diag = kt >= dstart
                ks = slice(kt * 128, (kt + 1) * 128)
                kb.op('pe', lambda e: e.matmul(ps[sbk][:], lhsT=kTh[hb][:, ks], rhs=qTh[hb][:, qs],
                                               start=True, stop=False),
                      reads=[f'kTh{hb}', f'qTh{hb}'], writes=[f'ps{sbk}'])
                if mla:
                    kb.op('pe', lambda e: e.matmul(ps[sbk][:], lhsT=krs[:, ks], rhs=qrh[hb][:, qs],
                                                   start=False, stop=not diag),
                          reads=['krs', f'qrh{hb}'], writes=[f'ps{sbk}'])
                else:
                    kb.op('pe', lambda e: e.matmul(ps[sbk][:], lhsT=selT[:, kt // 2, :], rhs=fmT[ob][:],
                                                   start=False, stop=not diag),
                          reads=['selT', f'fmT{ob}'], writes=[f'ps{sbk}'])
                if diag:
                    kb.op('pe', lambda e: e.matmul(ps[sbk][:], lhsT=ident_bf[:], rhs=dm_sb[:, kt - dstart, :],
                                                   start=False, stop=True),
                          reads=['ident_bf', 'dm'], writes=[f'ps{sbk}'])

            def prologue_a(h, j, ob):
                hb = h % 2
                if mla:
                    return
                for s in range(4):
                    kb.op('pe', lambda e, s=s: e.matmul(
                        ps[6][:, s * 32:(s + 1) * 32], lhsT=qTh[hb][:, j * T + s * 128: j * T + (s + 1) * 128],
                        rhs=kmT[:, h, :], start=True, stop=True),
                        reads=[f'qTh{hb}', 'kmT'], writes=['ps6'])
                tb = slice(j * 128, (j + 1) * 128)
                fl = "p a b -> p (a b)"
                kb.op('dve', lambda e: e.tensor_tensor(
                    out=gm[:].rearrange(fl), in0=ps[6][:, 0:128], in1=cbias[:, tb], op=ALU.add),
                    reads=['ps6', 'tabs'], writes=['gm'])
                for s in range(4):
                    kb.op('dve', lambda e, s=s: e.max(out=m8[:, s, :], in_=gm[:, s, :]),
                          reads=['gm'], writes=['m8'])
                for s in range(4):
                    kb.op('dve', lambda e, s=s: e.tensor_scalar(
                        out=selm[:, s, :], in0=gm[:, s, :], scalar1=m8[:, s, 2:3], scalar2=None,
                        op0=ALU.is_ge), reads=['gm', 'm8'], writes=['selm'])
                kb.op('dve', lambda e: e.tensor_tensor(
                    out=selm[:].rearrange(fl), in0=selm[:].rearrange(fl),
                    in1=aown[:, tb], op=ALU.max), reads=['selm', 'tabs'], writes=['selm'])
                kb.op('dve', lambda e: e.tensor_scalar(
                    out=selm[:].rearrange(fl), in0=selm[:].rearrange(fl),
                    scalar1=1.0, scalar2=BIG, op0=ALU.subtract, op1=ALU.mult),
                    reads=['selm'], writes=['selm'])
                kb.op('dve', lambda e: e.tensor_tensor(
                    out=fm[:].rearrange(fl), in0=selm[:].rearrange(fl),
                    in1=btab[:, tb], op=ALU.add), reads=['selm', 'tabs'], writes=['fm'])

            def prologue_b(h, j, ob):
                if mla:
                    return
                for s in range(4):
                    kb.op('pe', lambda e, s=s: e.transpose(
                        out=psb[0:32, s * 128:(s + 1) * 128], in_=fm[:, s, :], identity=ident_bf[:]),
                        reads=['fm', 'ident_bf'], writes=['psb'])
                kb.op('dve', lambda e: e.tensor_copy(out=fmT[ob][0:32, :], in_=psb[0:32, 0:T]),
                      reads=['psb'], writes=[f'fmT{ob}'])

            def mainloop(h, j, ob, nxt):
                hb = h % 2
                nkt, dstart = geom(j)
                po, pr = ps[2 + ob], ps[4 + ob]
                pok, prk = f'ps{2 + ob}', f'ps{4 + ob}'
                ka = max(0, nkt - 9)
                kbb = max(ka + 1, nkt - 3) if nkt > 4 else nkt - 1
                def pv(kt):
                    sbk = kt % 2
                    kb.op('pe', lambda e: e.matmul(po[:], lhsT=vh[hb][:, kt, :], rhs=pT[sbk][:],
                                                   start=(kt == 0), stop=(kt == nkt - 1)),
                          reads=[f'vh{hb}', f'pT{sbk}'], writes=[pok])
                    kb.op('pe', lambda e: e.matmul(pr[:], lhsT=ones_bf[:], rhs=pT[sbk][:],
                                                   start=(kt == 0), stop=(kt == nkt - 1)),
                          reads=['ones_bf', f'pT{sbk}'], writes=[prk])

                for kt in range(nkt):
                    sbk = kt % 2
                    if nxt is not None and kt == ka:
                        prologue_a(*nxt)
                    if nxt is not None and kt == kbb:
                        prologue_b(*nxt)
                    if kt + 1 < nkt:
                        qk(h, j, ob, kt + 1)
                    kb.op('act', lambda e, sbk=sbk: e.activation(out=pT[sbk][:], in_=ps[sbk][:], func=AF.Exp,
                                                                 scale=scale),
                          reads=[f'ps{sbk}'], writes=[f'pT{sbk}'])
                    if kt >= 1:
                        pv(kt - 1)
                return lambda: pv(nkt - 1)

            def epilogue(h, j, ob):
                po, pr = ps[2 + ob], ps[4 + ob]
                pok, prk = f'ps{2 + ob}', f'ps{4 + ob}'
                qs = slice(j * T, (j + 1) * T)
                kb.op('dve', lambda e: e.reciprocal(out=rinv[:], in_=pr[:]), reads=[prk], writes=['rinv'])
                kb.op('dve', lambda e: e.tensor_tensor(out=ost[ob][:], in0=po[:], in1=rinv[:], op=ALU.mult),
                      reads=[pok, 'rinv'], writes=[f'ost{ob}'])
                kb.dma('pool', oT[h * 128:(h + 1) * 128, qs], ost[ob][:], reads=[f'ost{ob}'], writes=['oT'])

            items = [(h, j) for h in range(8) for j in range(NQT)]
            load_head(0)
            prologue_a(items[0][0], items[0][1], 0)
            prologue_b(items[0][0], items[0][1], 0)
            qk(items[0][0], items[0][1], 0, 0)
            for idx, (h, j) in enumerate(items):
                ob = idx % 2
                if j == 0 and h + 1 < 8:
                    load_head(h + 1)
                nxt = None
                if idx + 1 < len(items):
                    nxt = (items[idx + 1][0], items[idx + 1][1], (idx + 1) % 2)
                finish = mainloop(h, j, ob, nxt)
                if nxt is not None:
                    qk(nxt[0], nxt[1], nxt[2], 0)
                finish()
                epilogue(h, j, ob)

        def rowlocal(layer, h_src, wo_ap, pT_ap, es2, nst, h_dst):
            def sb2(name, shape, dt):
                return es2.enter_context(nc.sbuf_tensor(uname(name), list(shape), dt))
            ST = 2048
            hs = sb2('hs', [128, 8, ST], F32)
            a32 = sb2('a32', [128, 8, ST], BF16)
            wbig = sb2('wbig', [128, 8, 1024], BF16)
            wpp = sb2('wpp', [128, 2, 1024], BF16)
            wgs = [sb2(f'wgs{i}', [128, 8, 256], BF16) for i in range(2)]
            wus = [sb2(f'wus{i}', [128, 8, 256], BF16) for i in range(2)]
            wds = [sb2(f'wds{i}', [128, 2, 1024], BF16) for i in range(2)]
            wr = sb2('wr', [128, 8, 20], F32)
            xnf = sb2('xnf', [128, 8, T], F32)
            combT = sb2('combT', [16, ST], BF16)
            lg = sb2('lg', [128, 20], F32)
            sm = sb2('sm', [128, 64], F32)
            em = sb2('em', [128, 16], F32)
            ee = sb2('ee', [128, 20], F32)
            selr = sb2('selr', [128, 16], F32)
            combs = [sb2(f'comb{i}', [128, 16], F32) for i in range(2)]
            m8r = sb2('m8r', [128, 8], F32)
            sa = [sb2(f'sa{i}', [128, T], F32) for i in range(2)]
            tt = [sb2(f'tt{i}', [128, T], F32) for i in range(2)]
            actb = [sb2(f'actb{i}', [128, 2, T], BF16) for i in range(2)]
            pf = sb2('pf', [128, 2, T], F32)
            ytmp = [pf[:, 0, :], pf[:, 1, :]]
            pbf = sb2('pbf', [128, 2, T], BF16)
            of = xnf
            kb.dma('sp', wr[:], I[f'wr{layer}'].rearrange("(c p) n -> p c n", p=128), writes=['wr'])
            ga, gf, gp = layer, 2 + layer, 4 + layer
            for st in range(nst):
                ts0 = st * ST
                for c in range(8):
                    kb.dma('sp', a32[:, c, :], oT[c * 128:(c + 1) * 128, ts0:ts0 + ST], reads=['oT'], writes=['a32'])
                load_w(wbig, 'wbig', wo_ap, 8, 1024, ceng='act')
                for c in range(8):
                    kb.dma('sp', hs[:, c, :], h_src[c * 128:(c + 1) * 128, ts0:ts0 + ST], reads=['hsrc'], writes=['hs'])
                n = 0
                for sub in range(4):
                    ss = slice(sub * T, (sub + 1) * T)
                    for oc in range(8):
                        b = n % 2
                        n += 1
                        for kc in range(8):
                            kb.op('pe', lambda e, kc=kc, oc=oc, b=b: e.matmul(
                                ps[b][:], lhsT=wbig[:, kc, oc * 128:(oc + 1) * 128], rhs=a32[:, kc, ss],
                                start=(kc == 0), stop=(kc == 7)), reads=['wbig', 'a32'], writes=[f'ps{b}'])
                        kb.op('dve', lambda e, oc=oc, b=b: e.tensor_tensor(
                            out=hs[:, oc, ss], in0=ps[b][:], in1=hs[:, oc, ss], op=ALU.add),
                            reads=[f'ps{b}', 'hs'], writes=['hs'])
                def fnorm_p1(sub):
                    ss = slice(sub * T, (sub + 1) * T)
                    rmsnorm_p1(lambda c: hs[:, c, ss], 'hs', 8, 1024.0, 6, sub % 2)

                def fnorm_p2(sub):
                    ss = slice(sub * T, (sub + 1) * T)
                    rmsnorm_p2(lambda c: hs[:, c, ss], 'hs', 8, lambda c: gains[:, gf, c:c + 1],
                               lambda c: a32[:, c, ss], 'a32', sub % 2,
                               dst_f_fn=lambda c: xnf[:, c, :], dfkey='xnf')

                LB = [(ps[5], 'ps5'), (ps[2], 'ps2')]
                TB = [(ps[4], 'ps4'), (ps[3], 'ps3')]

                def router_logits(s4):
                    lb, lk = LB[s4 % 2]
                    for kc in range(8):
                        kb.op('pe', lambda e, kc=kc: e.matmul(
                            lb[:, 0:20], lhsT=xnf[:, kc, s4 * 128:(s4 + 1) * 128], rhs=wr[:, kc, :],
                            start=(kc == 0), stop=(kc == 7)), reads=['xnf', 'wr'], writes=[lk])

                def router(sub):
                    router_logits(0)
                    for s4 in range(4):
                        if s4 + 1 < 4:
                            router_logits(s4 + 1)
                        lb, lk = LB[s4 % 2]
                        tb_, tk = TB[s4 % 2]
                        comb = combs[s4 % 2]
                        ck = f'comb{s4 % 2}'
                        D = 'dve'
                        kb.op(D, lambda e: e.tensor_copy(out=lg[:], in_=lb[:, 0:20]), reads=[lk], writes=['lg'])
                        kb.op(D, lambda e: e.tensor_reduce(out=sm[:, 0:1], in_=lg[:, 0:4], axis=AX.X, op=ALU.max),
                              reads=['lg'], writes=['sm'])
                        kb.op(D, lambda e: e.tensor_scalar(out=ee[:, 0:4], in0=lg[:, 0:4], scalar1=sm[:, 0:1],
                                                           scalar2=None, op0=ALU.subtract), reads=['lg', 'sm'], writes=['ee'])
                        kb.op(D, lambda e: e.tensor_scalar(out=sm[:, 12:16], in0=lg[:, 0:4], scalar1=sm[:, 0:1],
                                                           scalar2=None, op0=ALU.is_ge), reads=['lg', 'sm'], writes=['sm'])
                        kb.op(D, lambda e: e.tensor_scalar(out=sm[:, 12:16], in0=sm[:, 12:16], scalar1=1.0, scalar2=1e30,
                                                           op0=ALU.subtract, op1=ALU.mult), reads=['sm'], writes=['sm'])
                        for g in range(4):
                            kb.op(D, lambda e, g=g: e.tensor_scalar(
                                out=em[:, 4 * g:4 * g + 4], in0=lg[:, 4 + 4 * g:8 + 4 * g], scalar1=sm[:, 12 + g:13 + g],
                                scalar2=None, op0=ALU.add), reads=['lg', 'sm'], writes=['em'])
                        kb.op(D, lambda e: e.max(out=m8r[:], in_=em[:]), reads=['em'], writes=['m8r'])
                        kb.op(D, lambda e: e.tensor_scalar(out=selr[:], in0=em[:], scalar1=m8r[:, 1:2], scalar2=None,
                                                           op0=ALU.is_ge), reads=['em', 'm8r'], writes=['selr'])
                        kb.op(D, lambda e: e.tensor_scalar(out=ee[:, 4:20], in0=em[:], scalar1=m8r[:, 0:1], scalar2=-80.0,
                                                           op0=ALU.subtract, op1=ALU.max), reads=['em', 'm8r'], writes=['ee'])
                        kb.op('act', lambda e: e.activation(out=ee[:], in_=ee[:], func=AF.Exp), reads=['ee'], writes=['ee'])
                        kb.op(D, lambda e: e.tensor_reduce(out=sm[:, 1:2], in_=ee[:, 0:4], axis=AX.X, op=ALU.add),
                              reads=['ee'], writes=['sm'])
                        kb.op(D, lambda e: e.max(out=m8r[:], in_=ee[:, 4:20]), reads=['ee'], writes=['m8r'])
                        kb.op(D, lambda e: e.scalar_tensor_tensor(out=sm[:, 18:19], in0=m8r[:, 1:2], scalar=1.0,
                                                                  in1=sm[:, 1:2], op0=ALU.add, op1=ALU.mult),
                              reads=['sm', 'm8r'], writes=['sm'])
                        kb.op(D, lambda e: e.reciprocal(out=sm[:, 19:20], in_=sm[:, 18:19]), reads=['sm'], writes=['sm'])
                        kb.op(D, lambda e: e.scalar_tensor_tensor(out=comb[:], in0=ee[:, 4:20], scalar=sm[:, 19:20],
                                                                  in1=selr[:], op0=ALU.mult, op1=ALU.mult),
                              reads=['ee', 'sm', 'selr'], writes=[ck])
                        kb.op('pe', lambda e: e.transpose(out=tb_[0:16, 0:128], in_=comb[:], identity=ident_f[:]),
                              reads=[ck, 'ident_f'], writes=[tk])
                        c0 = sub * T + s4 * 128
                        kb.op('act', lambda e, c0=c0: e.activation(out=combT[:, c0:c0 + 128], in_=tb_[0:16, 0:128],
                                                                   func=AF.Copy), reads=[tk], writes=['combT'])

                fnorm_p1(0)
                fnorm_p2(0)
                for sub in range(4):
                    if sub + 1 < 4:
                        fnorm_p1(sub + 1)
                    router(sub)
                    if sub + 1 < 4:
                        fnorm_p2(sub + 1)
                stg_list = [(stg[0][:, :], 'stg0'), (stg[1][:, :], 'stg1')] + [
                    (xnf[:, 2 * i:2 * i + 2, :].rearrange("p a b -> p (a b)"), f'stgx{i}') for i in range(4)]

                def moe_piece(ex, p):
                    wb = ex % 2
                    sap, skey = stg_list[(6 * ex + p) % 6]
                    if p < 4:
                        wsrc = I[f'wg{layer}'] if p < 2 else I[f'wu{layer}']
                        half = p % 2
                        src = wsrc[ex][half * 512:(half + 1) * 512, :].rearrange("(c p) n -> p c n", p=128)
                        sview = sap.rearrange("p (c n) -> p c n", c=4)
                        dbuf, dkey = (wgs[wb], f'wgs{wb}') if p < 2 else (wus[wb], f'wus{wb}')
                        dst = dbuf[:, half * 4:(half + 1) * 4, :].rearrange("p c n -> p (c n)")
                    else:
                        fc = p - 4
                        src = I[f'wd{layer}'][ex][fc * 128:(fc + 1) * 128, :]
                        sview = sap
                        dst, dkey = wds[wb][:, fc, :], f'wds{wb}'
                    return sap, skey, sview, src, dst, dkey

                def moe_dma(ex, p):
                    if ex >= 16:
                        return
                    sap, skey, sview, src, dst, dkey = moe_piece(ex, p)
                    kb.dma('sp', sview, src, writes=[skey])

                def moe_cast(ex, p):
                    if ex >= 16:
                        return
                    sap, skey, sview, src, dst, dkey = moe_piece(ex, p)
                    kb.op('act', lambda e: e.activation(out=dst, in_=sap, func=AF.Copy), reads=[skey], writes=[dkey])

                def moe_comb(ex, sub):
                    ss = slice(sub * T, (sub + 1) * T)
                    kb.op('pe', lambda e: e.matmul(ps[0][:], lhsT=sel16[:, ex, :], rhs=combT[:, ss],
                                                   start=True, stop=True), reads=['sel16', 'combT'], writes=['ps0'])

                def moe_au_q(ex, sub, q):
                    wb = ex % 2
                    ss = slice(sub * T, (sub + 1) * T)
                    ab = sub % 2
                    fc, isup = q // 2, q % 2
                    pa, pu = 1 + fc, 3 + fc
                    if not isup:
                        for kc in range(8):
                            kb.op('pe', lambda e, kc=kc: e.matmul(
                                ps[pa][:], lhsT=wgs[wb][:, kc, fc * 128:(fc + 1) * 128], rhs=a32[:, kc, ss],
                                start=(kc == 0), stop=(kc == 7)), reads=[f'wgs{wb}', 'a32'], writes=[f'ps{pa}'])
                        kb.op('act', lambda e: e.activation(out=sa[fc][:], in_=ps[pa][:], func=AF.Silu),
                              reads=[f'ps{pa}'], writes=[f'sa{fc}'])
                    else:
                        for kc in range(8):
                            kb.op('pe', lambda e, kc=kc: e.matmul(
                                ps[pu][:], lhsT=wus[wb][:, kc, fc * 128:(fc + 1) * 128], rhs=a32[:, kc, ss],
                                start=(kc == 0), stop=(kc == 7)), reads=[f'wus{wb}', 'a32'], writes=[f'ps{pu}'])
                        kb.op('dve', lambda e: e.tensor_tensor(out=tt[fc][:], in0=ps[pu][:], in1=sa[fc][:],
                                                               op=ALU.mult),
                              reads=[f'ps{pu}', f'sa{fc}'], writes=[f'tt{fc}'])
                        kb.op('dve', lambda e: e.tensor_tensor(out=actb[ab][:, fc, :], in0=ps[0][:], in1=tt[fc][:],
                                                               op=ALU.mult),
                              reads=['ps0', f'tt{fc}'], writes=[f'actb{ab}'])

                def moe_down_q(ex, sub, q):
                    wb = ex % 2
                    ss = slice(sub * T, (sub + 1) * T)
                    ab = sub % 2
                    for oc in (2 * q, 2 * q + 1):
                        pb = 5 + (oc % 2)
                        for fc in range(2):
                            kb.op('pe', lambda e, fc=fc, oc=oc, pb=pb: e.matmul(
                                ps[pb][:], lhsT=wds[wb][:, fc, oc * 128:(oc + 1) * 128], rhs=actb[ab][:, fc, :],
                                start=(fc == 0), stop=(fc == 1)), reads=[f'wds{wb}', f'actb{ab}'], writes=[f'ps{pb}'])
                        if oc in (3, 7):
                            yb = (oc // 4) % 2
                            kb.op('act', lambda e, pb=pb, yb=yb: e.activation(out=ytmp[yb], in_=ps[pb][:], func=AF.Copy),
                                  reads=[f'ps{pb}'], writes=[f'ytmp{yb}'])
                            kb.op('pool', lambda e, oc=oc, yb=yb: e.tensor_tensor(
                                out=hs[:, oc, ss], in0=hs[:, oc, ss], in1=ytmp[yb], op=ALU.add),
                                reads=[f'ytmp{yb}', f'hs{oc}'], writes=[f'hs{oc}'])
                        else:
                            kb.op('dve', lambda e, oc=oc, pb=pb: e.tensor_tensor(
                                out=hs[:, oc, ss], in0=ps[pb][:], in1=hs[:, oc, ss], op=ALU.add),
                                reads=[f'ps{pb}', f'hs{oc}'], writes=[f'hs{oc}'])

                mitems = [(ex, sub) for ex in range(16) for sub in range(4)]
                kb.op('dve', lambda e: e.tensor_copy(out=sm[:, 41:42], in_=sm[:, 41:42]),
                      reads=['hs', 'sm', 'xnf'],
                      writes=['hs', 'sm', 'xnf'] + [f'hs{oc}' for oc in range(8)] + [f'stgx{i}' for i in range(4)]
                      + [f'xnf_{c}' for c in range(8)])
                for p in range(6):
                    moe_dma(0, p)
                    moe_cast(0, p)
                moe_dma(1, 0)
                moe_dma(1, 1)
                moe_comb(0, 0)
                for q in range(4):
                    moe_au_q(0, 0, q)
                for mi, (ex, sub) in enumerate(mitems):
                    nxt = mitems[mi + 1] if mi + 1 < len(mitems) else None
                    if nxt is not None:
                        moe_comb(*nxt)
                    for q in range(4):
                        if nxt is not None:
                            moe_au_q(nxt[0], nxt[1], q)
                        moe_down_q(ex, sub, q)
                    if sub == 3:
                        moe_dma(ex + 2, 0)
                        moe_dma(ex + 2, 1)
                    else:
                        moe_cast(ex + 1, 2 * sub)
                        moe_cast(ex + 1, 2 * sub + 1)
                        if sub < 2:
                            moe_dma(ex + 1, 2 * sub + 2)
                            moe_dma(ex + 1, 2 * sub + 3)
                PK = [f'a32p{i}' for i in range(4)] + [f'hsp{i}' for i in range(4)] + [f'ofc{c}' for c in range(8)]
                kb.op('dve', lambda e: e.tensor_copy(out=sm[:, 40:41], in_=sm[:, 40:41]),
                      reads=[f'hs{oc}' for oc in range(8)] + ['sm', 'a32'] + [f'stgx{i}' for i in range(4)],
                      writes=['hs', 'sm', 'xnf', 'a32'] + [f'stgx{i}' for i in range(4)] + PK)
                load_w(wbig, 'wbig', I[f'pg{layer}'], 8, 1024, ceng='act')
                load_w(wpp, 'wpp', I[f'pp{layer}'], 2, 1024, ceng='act')

                def ple_load(sub):
                    for c2 in range(2):
                        kb.dma('sp', pf[:, c2, :], pT_ap[c2 * 128:(c2 + 1) * 128, ts0 + sub * T: ts0 + (sub + 1) * T],
                               writes=['pf', 'ytmp0', 'ytmp1'])

                def ple_cast(sub):
                    kb.op('act', lambda e: e.activation(out=pbf[:].rearrange("p a b -> p (a b)"),
                                                        in_=pf[:].rearrange("p a b -> p (a b)"), func=AF.Copy),
                          reads=['pf'], writes=['pbf'])

                def ple_p1(sub):
                    ss = slice(sub * T, (sub + 1) * T)
                    rmsnorm_p1(lambda c: hs[:, c, ss], f'hsp{sub}', 8, 1024.0, 6)

                def ple_p2(sub):
                    ss = slice(sub * T, (sub + 1) * T)
                    rmsnorm_p2(lambda c: hs[:, c, ss], f'hsp{sub}', 8, lambda c: gains[:, gp, c:c + 1],
                               lambda c: a32[:, c, ss], f'a32p{sub}')

                def ple_oc(sub, oc):
                    ss = slice(sub * T, (sub + 1) * T)
                    pg, pq = (oc % 2), 2 + (oc % 2)
                    fc = oc % 2
                    for kc in range(8):
                        kb.op('pe', lambda e, kc=kc: e.matmul(
                            ps[pg][:], lhsT=wbig[:, kc, oc * 128:(oc + 1) * 128], rhs=a32[:, kc, ss],
                            start=(kc == 0), stop=(kc == 7)), reads=['wbig', f'a32p{sub}'], writes=[f'ps{pg}'])
                    for c2 in range(2):
                        kb.op('pe', lambda e, c2=c2: e.matmul(
                            ps[pq][:], lhsT=wpp[:, c2, oc * 128:(oc + 1) * 128], rhs=pbf[:, c2, :],
                            start=(c2 == 0), stop=(c2 == 1)), reads=['wpp', 'pbf'], writes=[f'ps{pq}'])
                    kb.op('act', lambda e: e.activation(out=sa[fc][:], in_=ps[pg][:], func=AF.Sigmoid),
                          reads=[f'ps{pg}'], writes=[f'sa{fc}'])
                    kb.op('dve', lambda e: e.tensor_tensor(out=tt[fc][:], in0=ps[pq][:], in1=sa[fc][:], op=ALU.mult),
                          reads=[f'ps{pq}', f'sa{fc}'], writes=[f'tt{fc}'])
                    kb.op('pool', lambda e: e.tensor_tensor(out=hs[:, oc, ss], in0=hs[:, oc, ss],
                                                            in1=tt[fc][:], op=ALU.add),
                          reads=[f'hsp{sub}', f'tt{fc}'], writes=[f'hsp{sub}'])

                def ple_final(sub):
                    ss = slice(sub * T, (sub + 1) * T)
                    hk = f'hsp{sub}'
                    for c in range(8):
                        kb.op('act', lambda e, c=c: e.activation(out=sq[:, c, :], in_=hs[:, c, ss], func=AF.Square),
                              reads=[hk], writes=['sq'])
                    for c in range(8):
                        kb.op('pe', lambda e, c=c: e.matmul(ps[6][:], lhsT=ones_bf[:], rhs=sq[:, c, :],
                                                            start=(c == 0), stop=(c == 7)),
                              reads=['sq', 'ones_bf'], writes=['ps6'])
                    kb.op('act', lambda e: e.activation(out=lnv[:], in_=ps[6][:], func=AF.Ln, scale=1.0 / 1024.0,
                                                        bias=eps_t[:, 0:1]), reads=['ps6', 'eps'], writes=['lnv'])
                    kb.op('act', lambda e: e.activation(out=rs_b[:], in_=lnv[:], func=AF.Exp, scale=-0.5),
                          reads=['lnv'], writes=['rs1'])
                    for c in range(8):
                        kb.op('dve', lambda e, c=c: e.scalar_tensor_tensor(
                            out=of[:, c, :], in0=hs[:, c, ss], scalar=gains[:, 6, c:c + 1], in1=rs_b[:],
                            op0=ALU.mult, op1=ALU.mult), reads=[hk, 'rs1', 'gains'], writes=[f'ofc{c}'])
                    for c in range(8):
                        kb.dma('pool', outT[c * 128:(c + 1) * 128, ts0 + sub * T: ts0 + (sub + 1) * T], of[:, c, :],
                               reads=[f'ofc{c}'], writes=['outT'])

                P1EARLY = True
                ple_load(0)
                ple_cast(0)
                ple_p1(0)
                for sub in range(4):
                    more = sub + 1 < 4
                    if sub > 0:
                        ple_load(sub)
                        ple_cast(sub)
                        if not P1EARLY:
                            ple_p1(sub)
                    ple_p2(sub)
                    for oc in range(0, 4):
                        ple_oc(sub, oc)
                    if more and P1EARLY:
                        ple_p1(sub + 1)
                    for oc in range(4, 8):
                        ple_oc(sub, oc)
                    if layer == 1:
                        ple_final(sub)
                    else:
                        ss = slice(sub * T, (sub + 1) * T)
                        for c in range(8):
                            kb.dma('pool', h_dst[c * 128:(c + 1) * 128, ts0 + sub * T:ts0 + (sub + 1) * T], hs[:, c, ss],
                                   reads=[f'hsp{sub}'], writes=['hT'])
                kb.op('dve', lambda e: e.tensor_copy(out=sm[:, 42:43], in_=sm[:, 42:43]),
                      reads=['sm'] + PK, writes=['hs', 'sm', 'xnf', 'a32'] + PK)

        with ExitStack() as es2:
            def sb2(name, shape, dt):
                return es2.enter_context(nc.sbuf_tensor(uname(name), list(shape), dt))
            kmT = sb('kmT', [128, 8, 32], BF16)
            kms = sb2('kms', [128, 8, 32], F32)
            w1 = sb2('w1', [128, 8, 1024], BF16)
            w1s = sb2('w1s', [128, 8, 1024], BF16)
            w2 = sb2('w2', [128, 8, 1024], BF16)
            w3 = sb2('w3', [128, 8, 1024], BF16)
            w3s = sb2('w3s', [128, 8, 1024], BF16)
            xs = [sb2(f'xs{i}', [128, 8, T], F32) for i in range(2)]
            xn = [sb2(f'xn{i}', [128, 8, T], BF16) for i in range(2)]
            cs = [sb2(f'cs{i}', [128, T], F32) for i in range(2)]
            sn = [sb2(f'sn{i}', [128, T], F32) for i in range(2)]
            t1 = [sb2(f't1{i}', [128, T], F32) for i in range(2)]
            t2 = [sb2(f't2{i}', [128, T], F32) for i in range(2)]
            kf = [sb2(f'kf{i}', [128, T], F32) for i in range(2)]
            kbf = sb2('kbf', [128, 8, T], BF16)
            qbf = sb2('qbf', [128, 8, T], BF16)
            vbf = sb2('vbf', [128, 4, 1024], BF16)

            def proj_rope(b, g, wa, was, wk_, wsk_, obuf, okey, dst_dram, dkey, with_kmean, h0, h1):
                gs = slice(g * T, (g + 1) * T)
                for h in range(h0, h1):
                    hb = h % 2
                    p1, p2 = hb, 2 + hb
                    for kc in range(8):
                        kb.op('pe', lambda e, kc=kc: e.matmul(ps[p1][:], lhsT=wa[:, kc, h * 128:(h + 1) * 128],
                                                              rhs=xn[b][:, kc, :], start=(kc == 0), stop=(kc == 7)),
                              reads=[wk_, f'xn{b}'], writes=[f'ps{p1}'])
                    for kc in range(8):
                        kb.op('pe', lambda e, kc=kc: e.matmul(ps[p2][:], lhsT=was[:, kc, h * 128:(h + 1) * 128],
                                                              rhs=xn[b][:, kc, :], start=(kc == 0), stop=(kc == 7)),
                              reads=[wsk_, f'xn{b}'], writes=[f'ps{p2}'])
                    kb.op('dve', lambda e: e.tensor_tensor(out=t1[hb][:], in0=ps[p1][:], in1=cs[b][:], op=ALU.mult),
                          reads=[f'ps{p1}', f'cs{b}'], writes=[f't1{hb}'])
                    kb.op('dve', lambda e: e.tensor_tensor(out=t2[hb][:], in0=ps[p2][:], in1=sn[b][:], op=ALU.mult),
                          reads=[f'ps{p2}', f'sn{b}'], writes=[f't2{hb}'])
                    if with_kmean:
                        kb.op('pool', lambda e: e.tensor_tensor(out=kf[hb][:], in0=t1[hb][:], in1=t2[hb][:], op=ALU.add),
                              reads=[f't1{hb}', f't2{hb}'], writes=[f'kf{hb}'])
                        kb.op('dve', lambda e: e.tensor_reduce(
                            out=kms[:, h, 2 * g:2 * g + 2], in_=kf[hb][:].rearrange("p (a b) -> p a b", a=2),
                            axis=AX.X, op=ALU.add), reads=[f'kf{hb}'], writes=['kms'])
                        kb.op('act', lambda e: e.activation(out=obuf[:, h, :], in_=kf[hb][:], func=AF.Copy),
                              reads=[f'kf{hb}'], writes=[okey])
                    else:
                        kb.op('pool', lambda e: e.tensor_tensor(out=obuf[:, h, :], in0=t1[hb][:], in1=t2[hb][:], op=ALU.add),
                              reads=[f't1{hb}', f't2{hb}'], writes=[okey])
                if h1 == 8:
                    kb.dma('pool', dst_dram[:, gs].rearrange("(h p) t -> p h t", p=128), obuf[:],
                           reads=[okey], writes=[dkey])

            def load_x(b, g):
                gs = slice(g * T, (g + 1) * T)
                for c in range(8):
                    kb.dma('sp', xs[b][:, c, :], I['xT_all'][c * 128:(c + 1) * 128, gs], writes=[f'xs{b}'])
                kb.dma('sp', cs[b][:], I['cosg'][:, gs], writes=[f'cs{b}'])
                kb.dma('sp', sn[b][:], I['sing'][:, gs], writes=[f'sn{b}'])

            def norm_p1(b):
                rmsnorm_p1(lambda c: xs[b][:, c, :], f'xs{b}', 8, 1024.0, 6)

            def norm_p2(b):
                rmsnorm_p2(lambda c: xs[b][:, c, :], f'xs{b}', 8, lambda c: gains[:, 0, c:c + 1],
                           lambda c: xn[b][:, c, :], f'xn{b}')

            load_w(w1, 'w1', I['wk0'], 8, 1024, ceng='act')
            load_x(0, 0)
            load_w(w1s, 'w1s', I['wk0s'], 8, 1024, ceng='act')
            norm_p1(0)
            norm_p2(0)
            load_w(w2, 'w2', I['wv0'], 8, 1024)
            load_w(w3, 'w3', I['wq0'], 8, 1024)
            load_w(w3s, 'w3s', I['wq0s'], 8, 1024)
            for g in range(NGC):
                b = g % 2
                nb_ = 1 - b
                if g + 1 < NGC:
                    load_x(nb_, g + 1)
                proj_rope(b, g, w1, w1s, 'w1', 'w1s', kbf, 'kbf', kT, 'kT', True, 0, 4)
                if g + 1 < NGC:
                    norm_p1(nb_)
                proj_rope(b, g, w1, w1s, 'w1', 'w1s', kbf, 'kbf', kT, 'kT', True, 4, 8)
                for s_ in range(4):
                    for half in range(2):
                        pv = 4 + half
                        for kc in range(8):
                            kb.op('pe', lambda e, kc=kc, s_=s_, half=half, pv=pv: e.matmul(
                                ps[pv][:], lhsT=xn[b][:, kc, s_ * 128:(s_ + 1) * 128],
                                rhs=w2[:, kc, half * 512:(half + 1) * 512], start=(kc == 0), stop=(kc == 7)),
                                reads=['w2', f'xn{b}'], writes=[f'ps{pv}'])
                        kb.op('act', lambda e, s_=s_, half=half, pv=pv: e.activation(
                            out=vbf[:, s_, half * 512:(half + 1) * 512], in_=ps[pv][:], func=AF.Copy),
                            reads=[f'ps{pv}'], writes=[f'vbf{s_}'])
                    kb.dma('pool', vdr[:, :, 4 * g + s_, :].rearrange("h p d -> p h d"),
                           vbf[:, s_, :].rearrange("p (h d) -> p h d", h=8),
                           reads=[f'vbf{s_}'], writes=['vdr'])
                if g + 1 < NGC:
                    norm_p2(nb_)
                proj_rope(b, g, w3, w3s, 'w3', 'w3s', qbf, 'qbf', qT0, 'qsrc', False, 0, 8)
            kb.op('dve', lambda e: e.tensor_scalar(out=kmT[:].rearrange("p a b -> p (a b)"),
                                                   in0=kms[:].rearrange("p a b -> p (a b)"),
                                                   scalar1=1.0 / 256.0, scalar2=None, op0=ALU.mult),
                  reads=['kms'], writes=['kmT'])
        kb.barrier()
        with ExitStack() as es2:
            cbias = es2.enter_context(nc.sbuf_tensor(uname('cbias'), [128, NGC * 128], F32))
            aown = es2.enter_context(nc.sbuf_tensor(uname('aown'), [128, NGC * 128], F32))
            btab = es2.enter_context(nc.sbuf_tensor(uname('btab'), [128, NGC * 128], F32))
            selT = es2.enter_context(nc.sbuf_tensor(uname('selT'), [128, 32, 128], BF16))
            dm0 = es2.enter_context(nc.sbuf_tensor(uname('dm0'), [128, 4, T], BF16))
            kb.dma('sp', cbias[:], I['cbias'], writes=['tabs'])
            kb.dma('sp', aown[:], I['aown'], writes=['tabs'])
            kb.dma('sp', btab[:], I['btab'], writes=['tabs'])
            kb.op('pool', lambda e: e.memset(selT[:], 0.0), writes=['selT'])
            kb.dma('sp', selT[0:32, :, :], I['selT'], writes=['selT'])
            kb.dma('sp', dm0[:], I['dm0'], writes=['dm'])
            attention(0, 128.0 ** -0.5, dm0, es2)
        kb.barrier()
        with ExitStack() as es2:
            rowlocal(0, I['xT_all'], I['wo0'], I['p0T'], es2, 4, hT_all)
        kb.barrier()
        with ExitStack() as es2:
            def sb2(name, shape, dt):
                return es2.enter_context(nc.sbuf_tensor(uname(name), list(shape), dt))
            wdkvc = sb2('wdkvc', [128, 8, 256], BF16)
            wdkvr = sb2('wdkvr', [128, 8, 64], BF16)
            wdkvrs = sb2('wdkvrs', [128, 8, 64], BF16)
            wk = sb2('wk', [128, 2, 1024], BF16)
            wv = sb2('wv', [128, 2, 1024], BF16)
            kvn_t = sb2('kvn_t', [128, 2], F32)
            xs = [sb2(f'xs{i}', [128, 8, T], F32) for i in range(2)]
            xn = [sb2(f'xn{i}', [128, 8, T], BF16) for i in range(2)]
            ckf = sb2('ckf', [128, 2, T], F32)
            ckn = [sb2(f'ckn{i}', [128, 2, T], BF16) for i in range(2)]
            cs = [sb2(f'cs{i}', [128, T], F32) for i in range(2)]
            sn = [sb2(f'sn{i}', [128, T], F32) for i in range(2)]
            t1 = [sb2(f't1{i}', [128, T], F32) for i in range(2)]
            t2 = [sb2(f't2{i}', [128, T], F32) for i in range(2)]
            krb = [sb2(f'krb{i}', [64, T], BF16) for i in range(2)]
            kbf = [sb2(f'kbf{i}', [128, 8, T], BF16) for i in range(2)]
            vbf = [sb2(f'vbf{i}', [128, 4, 1024], BF16) for i in range(2)]
            kb.dma('sp', kvn_t[:], I['kvn'], writes=['qkvn'])
            load_w(wdkvc, 'wdkvc', I['wdkvc'], 8, 256)
            load_w(wdkvr, 'wdkvr', I['wdkvr'], 8, 64)
            load_w(wdkvrs, 'wdkvrs', I['wdkvrs'], 8, 64)
            load_w(wk, 'wk', I['wukvk'], 2, 1024)
            load_w(wv, 'wv', I['wukvv'], 2, 1024)
            def kv_load(b, g):
                gs = slice(g * T, (g + 1) * T)
                for c in range(8):
                    kb.dma('sp', xs[b][:, c, :], hT_all[c * 128:(c + 1) * 128, gs], reads=['hT'], writes=[f'xs{b}'])
                kb.dma('sp', cs[b][:], I['cos64g'][:, gs], writes=[f'cs{b}'])
                kb.dma('sp', sn[b][:], I['sin64g'][:, gs], writes=[f'sn{b}'])

            def kv_p1(b):
                rmsnorm_p1(lambda c: xs[b][:, c, :], f'xs{b}', 8, 1024.0, 6)

            def kv_p2(b):
                rmsnorm_p2(lambda c: xs[b][:, c, :], f'xs{b}', 8, lambda c: gains[:, 1, c:c + 1],
                           lambda c: xn[b][:, c, :], f'xn{b}')

            kv_load(0, 0)
            kv_p1(0)
            kv_p2(0)
            for g in range(NGC):
                b = g % 2
                gs = slice(g * T, (g + 1) * T)
                if g + 1 < NGC:
                    kv_load(1 - b, g + 1)
                for oc in range(2):
                    pb = oc
                    for kc in range(8):
                        kb.op('pe', lambda e, kc=kc, oc=oc, pb=pb: e.matmul(
                            ps[pb][:], lhsT=wdkvc[:, kc, oc * 128:(oc + 1) * 128], rhs=xn[b][:, kc, :],
                            start=(kc == 0), stop=(kc == 7)), reads=['wdkvc', f'xn{b}'], writes=[f'ps{pb}'])
                    kb.op('act', lambda e, oc=oc, pb=pb: e.activation(out=ckf[:, oc, :], in_=ps[pb][:], func=AF.Copy),
                          reads=[f'ps{pb}'], writes=['ckf'])
                rmsnorm(lambda c: ckf[:, c, :], 'ckf', 2, lambda c: kvn_t[:, c:c + 1],
                        lambda c: ckn[b][:, c, :], f'ckn{b}', 256.0, 6)
                if g + 1 < NGC:
                    kv_p1(1 - b)
                for kc in range(8):
                    kb.op('pe', lambda e, kc=kc: e.matmul(ps[2][0:64, :], lhsT=wdkvr[:, kc, :], rhs=xn[b][:, kc, :],
                                                          start=(kc == 0), stop=(kc == 7)),
                          reads=['wdkvr', f'xn{b}'], writes=['ps2'])
                for kc in range(8):
                    kb.op('pe', lambda e, kc=kc: e.matmul(ps[4][0:64, :], lhsT=wdkvrs[:, kc, :], rhs=xn[b][:, kc, :],
                                                          start=(kc == 0), stop=(kc == 7)),
                          reads=['wdkvrs', f'xn{b}'], writes=['ps4'])
                kb.op('dve', lambda e: e.tensor_tensor(out=t1[0][0:64, :], in0=ps[2][0:64, :], in1=cs[b][0:64, :],
                                                       op=ALU.mult), reads=['ps2', f'cs{b}'], writes=['t10'])
                kb.op('dve', lambda e: e.tensor_tensor(out=t2[0][0:64, :], in0=ps[4][0:64, :], in1=sn[b][0:64, :],
                                                       op=ALU.mult), reads=['ps4', f'sn{b}'], writes=['t20'])
                kb.op('pool', lambda e: e.tensor_tensor(out=krb[b][:], in0=t1[0][0:64, :], in1=t2[0][0:64, :],
                                                        op=ALU.add), reads=['t10', 't20'], writes=[f'krb{b}'])
                kb.dma('pool', krT[:, gs], krb[b][:], reads=[f'krb{b}'], writes=['krT'])
                for h in range(8):
                    pb = h % 2
                    for kc in range(2):
                        kb.op('pe', lambda e, kc=kc, h=h, pb=pb: e.matmul(
                            ps[pb][:], lhsT=wk[:, kc, h * 128:(h + 1) * 128], rhs=ckn[b][:, kc, :],
                            start=(kc == 0), stop=(kc == 1)), reads=['wk', f'ckn{b}'], writes=[f'ps{pb}'])
                    kb.op('act', lambda e, h=h, pb=pb: e.activation(out=kbf[b][:, h, :], in_=ps[pb][:], func=AF.Copy),
                          reads=[f'ps{pb}'], writes=[f'kbf{b}'])
                kb.dma('pool', kT[:, gs].rearrange("(h p) t -> p h t", p=128), kbf[b][:],
                       reads=[f'kbf{b}'], writes=['kT'])
                if g + 1 < NGC:
                    kv_p2(1 - b)
                for s_ in range(4):
                    for half in range(2):
                        pv = 2 + 2 * half
                        pv = 3 if half == 0 else 5
                        for kc in range(2):
                            kb.op('pe', lambda e, kc=kc, s_=s_, half=half, pv=pv: e.matmul(
                                ps[pv][:], lhsT=ckn[b][:, kc, s_ * 128:(s_ + 1) * 128],
                                rhs=wv[:, kc, half * 512:(half + 1) * 512], start=(kc == 0), stop=(kc == 1)),
                                reads=['wv', f'ckn{b}'], writes=[f'ps{pv}'])
                        kb.op('dve', lambda e, s_=s_, half=half, pv=pv: e.tensor_copy(
                            out=vbf[b][:, s_, half * 512:(half + 1) * 512], in_=ps[pv][:]),
                            reads=[f'ps{pv}'], writes=[f'vbf{b}_{s_}'])
                    kb.dma('pool', vdr[:, :, 4 * g + s_, :].rearrange("h p d -> p h d"),
                           vbf[b][:, s_, :].rearrange("p (h d) -> p h d", h=8),
                           reads=[f'vbf{b}_{s_}'], writes=['vdr'])
        kb.barrier()
        with ExitStack() as es2:
            def sb2(name, shape, dt):
                return es2.enter_context(nc.sbuf_tensor(uname(name), list(shape), dt))
            wdq = sb2('wdq', [128, 8, 384], BF16)
            wuqn = sb2('wuqn', [128, 3, 1024], BF16)
            wuqr = sb2('wuqr', [128, 3, 512], BF16)
            wuqrs = sb2('wuqrs', [128, 3, 512], BF16)
            qn_t = sb2('qn_t', [128, 3], F32)
            sel2 = sb2('sel2', [128, 2], F32)
            xs = [sb2(f'xs{i}', [128, 8, T], F32) for i in range(2)]
            xs2 = sb2('xs2', [128, 8, T], F32)
            xn = [sb2(f'xn{i}', [128, 8, T], BF16) for i in range(2)]
            cqf = sb2('cqf', [128, 3, T], F32)
            cqn = sb2('cqn', [128, 3, T], BF16)
            cs = [sb2(f'cs{i}', [128, T], F32) for i in range(2)]
            sn = [sb2(f'sn{i}', [128, T], F32) for i in range(2)]
            t1 = [sb2(f't1{i}', [128, T], F32) for i in range(2)]
            t2 = [sb2(f't2{i}', [128, T], F32) for i in range(2)]
            qnb = [sb2(f'qnb{i}', [128, 8, T], BF16) for i in range(2)]
            qrb = [sb2(f'qrb{i}', [128, 4, T], BF16) for i in range(2)]
            kb.dma('sp', qn_t[:], I['qn'], writes=['qkvn'])
            kb.dma('sp', sel2[:], I['sel2'], writes=['sel2'])
            load_w(wdq, 'wdq', I['wdq'], 8, 384)
            load_w(wuqn, 'wuqn', I['wuqn'], 3, 1024)
            load_w(wuqr, 'wuqr', I['wuqr'], 3, 512)
            load_w(wuqrs, 'wuqrs', I['wuqrs'], 3, 512)
            def q_load(b, j):
                js = slice(j * T, (j + 1) * T)
                ga = slice((2 * j) * T, (2 * j + 1) * T)
                gb_ = slice((2 * j + 1) * T, (2 * j + 2) * T)
                for c in range(8):
                    kb.dma('sp', xs[b][:, c, :], hT_all[c * 128:(c + 1) * 128, ga], reads=['hT'], writes=[f'xs{b}'])
                    kb.dma('sp', xs2[:, c, :], hT_all[c * 128:(c + 1) * 128, gb_], reads=['hT'], writes=['xs2'])
                kb.dma('sp', cs[b][:], I['cos64'][:, js], writes=[f'cs{b}'])
                kb.dma('sp', sn[b][:], I['sin64'][:, js], writes=[f'sn{b}'])

            def q_select(b, j):
                js = slice(j * T, (j + 1) * T)
                for c in range(8):
                    kb.op('dve', lambda e, c=c: e.tensor_scalar(out=xs[b][:, c, :], in0=xs[b][:, c, :], scalar1=sel2[:, 0:1],
                                                                scalar2=None, op0=ALU.mult),
                          reads=[f'xs{b}', 'sel2'], writes=[f'xs{b}'])
                    kb.op('dve', lambda e, c=c: e.scalar_tensor_tensor(
                        out=xs[b][:, c, :], in0=xs2[:, c, :], scalar=sel2[:, 1:2], in1=xs[b][:, c, :],
                        op0=ALU.mult, op1=ALU.add), reads=[f'xs{b}', 'xs2', 'sel2'], writes=[f'xs{b}'])
                for c in range(8):
                    kb.dma('sp', hT_loc[c * 128:(c + 1) * 128, js], xs[b][:, c, :], reads=[f'xs{b}'], writes=['hTl'])

            def q_p1(b):
                rmsnorm_p1(lambda c: xs[b][:, c, :], f'xs{b}', 8, 1024.0, 6)

            def q_p2(b):
                rmsnorm_p2(lambda c: xs[b][:, c, :], f'xs{b}', 8, lambda c: gains[:, 1, c:c + 1],
                           lambda c: xn[b][:, c, :], f'xn{b}')

            q_load(0, 0)
            q_select(0, 0)
            q_p1(0)
            q_p2(0)
            for j in range(NJ):
                b = j % 2
                js = slice(j * T, (j + 1) * T)
                if j + 1 < NJ:
                    q_load(1 - b, j + 1)
                for oc in range(3):
                    pb = oc % 2
                    for kc in range(8):
                        kb.op('pe', lambda e, kc=kc, oc=oc, pb=pb: e.matmul(
                            ps[pb][:], lhsT=wdq[:, kc, oc * 128:(oc + 1) * 128], rhs=xn[b][:, kc, :],
                            start=(kc == 0), stop=(kc == 7)), reads=['wdq', f'xn{b}'], writes=[f'ps{pb}'])
                    kb.op('act', lambda e, oc=oc, pb=pb: e.activation(out=cqf[:, oc, :], in_=ps[pb][:], func=AF.Copy),
                          reads=[f'ps{pb}'], writes=['cqf'])
                rmsnorm(lambda c: cqf[:, c, :], 'cqf', 3, lambda c: qn_t[:, c:c + 1],
                        lambda c: cqn[:, c, :], 'cqn', 384.0, 6)
                if j + 1 < NJ:
                    q_select(1 - b, j + 1)
                    q_p1(1 - b)
                for h in range(8):
                    pb = h % 2
                    for kc in range(3):
                        kb.op('pe', lambda e, kc=kc, h=h, pb=pb: e.matmul(
                            ps[pb][:], lhsT=wuqn[:, kc, h * 128:(h + 1) * 128], rhs=cqn[:, kc, :],
                            start=(kc == 0), stop=(kc == 2)), reads=['wuqn', 'cqn'], writes=[f'ps{pb}'])
                    kb.op('act', lambda e, h=h, pb=pb: e.activation(out=qnb[b][:, h, :], in_=ps[pb][:], func=AF.Copy),
                          reads=[f'ps{pb}'], writes=[f'qnb{b}'])
                kb.dma('pool', qnT[:, js].rearrange("(h p) t -> p h t", p=128), qnb[b][:],
                       reads=[f'qnb{b}'], writes=['qsrc'])
                if j + 1 < NJ:
                    q_p2(1 - b)
                for hp in range(4):
                    pb = hp % 2
                    p1, p2 = 2 + pb, 4 + pb
                    for kc in range(3):
                        kb.op('pe', lambda e, kc=kc, hp=hp, p1=p1: e.matmul(
                            ps[p1][:], lhsT=wuqr[:, kc, hp * 128:(hp + 1) * 128], rhs=cqn[:, kc, :],
                            start=(kc == 0), stop=(kc == 2)), reads=['wuqr', 'cqn'], writes=[f'ps{p1}'])
                    for kc in range(3):
                        kb.op('pe', lambda e, kc=kc, hp=hp, p2=p2: e.matmul(
                            ps[p2][:], lhsT=wuqrs[:, kc, hp * 128:(hp + 1) * 128], rhs=cqn[:, kc, :],
                            start=(kc == 0), stop=(kc == 2)), reads=['wuqrs', 'cqn'], writes=[f'ps{p2}'])
                    kb.op('dve', lambda e, pb=pb, p1=p1: e.tensor_tensor(out=t1[pb][:], in0=ps[p1][:], in1=cs[b][:],
                                                                         op=ALU.mult),
                          reads=[f'ps{p1}', f'cs{b}'], writes=[f't1{pb}'])
                    kb.op('dve', lambda e, pb=pb, p2=p2: e.tensor_tensor(out=t2[pb][:], in0=ps[p2][:], in1=sn[b][:],
                                                                         op=ALU.mult),
                          reads=[f'ps{p2}', f'sn{b}'], writes=[f't2{pb}'])
                    kb.op('pool', lambda e, pb=pb, hp=hp: e.tensor_tensor(out=qrb[b][:, hp, :], in0=t1[pb][:],
                                                                          in1=t2[pb][:], op=ALU.add),
                          reads=[f't1{pb}', f't2{pb}'], writes=[f'qrb{b}'])
                kb.dma('pool', qrT[:, js].rearrange("(h p) t -> p h t", p=128), qrb[b][:],
                       reads=[f'qrb{b}'], writes=['qrT'])
        kb.barrier()
        with ExitStack() as es2:
            dm1 = es2.enter_context(nc.sbuf_tensor(uname('dm1'), [128, 8, T], BF16))
            kb.dma('sp', dm1[:], I['dm1'], writes=['dm'])
            attention(1, 192.0 ** -0.5, dm1, es2)
        kb.barrier()
        with ExitStack() as es2:
            rowlocal(1, hT_loc, I['wo1'], I['p1T'], es2, 2, None)
        kb.barrier()
    return nc


def _rope_tables(pos, dim):
    half = dim // 2
    inv = (np.float32(10000.0) ** (-np.arange(half, dtype=np.float32) / np.float32(half))).astype(np.float32)
    ang = (pos.astype(np.float32)[:, None] * inv[None, :]).astype(np.float32)
    cos, sin = np.cos(ang).astype(np.float32), np.sin(ang).astype(np.float32)
    cosT = np.concatenate([cos, cos], axis=1).T
    sinT = np.concatenate([-sin, sin], axis=1).T
    return np.ascontiguousarray(cosT), np.ascontiguousarray(sinT)


def _swap_halves(w, n_heads, dim):
    k = w.shape[0]
    w4 = w.reshape(k, n_heads, 2, dim // 2)
    return np.ascontiguousarray(w4[:, :, ::-1, :].reshape(k, n_heads * dim))


def _consts():
    c = {}
    c['ones_bf'] = np.ones((128, 128), NPBF)
    c['ident_bf'] = np.eye(128, dtype=np.float32).astype(NPBF)
    c['ident_f'] = np.eye(128, dtype=np.float32)
    s16 = np.zeros((16, 16, 128), np.float32)
    for n in range(16):
        s16[n, n, :] = 1.0
    c['sel16'] = s16.astype(NPBF)
    s32 = np.zeros((32, 32, 128), np.float32)
    for n in range(32):
        s32[n, n, :] = 1.0
    c['selT'] = s32.astype(NPBF)
    return c


def _moba_tables():
    t = {}
    cb = np.zeros((128, NGC, 4, 32), np.float32)
    ao = np.zeros((128, NGC, 4, 32), np.float32)
    bt = np.zeros((128, NGC, 4, 32), np.float32)
    n = np.arange(32)
    for g in range(NGC):
        for s in range(4):
            own = g * 2 + s // 2
            cb[:, g, s, :] = np.where(n < own, 0.0, NEG)[None, :]
            ao[:, g, s, :] = (n == own).astype(np.float32)[None, :]
            bt[:, g, s, :] = np.where(n > own, -BIG, 0.0)[None, :]
    t['cbias'] = cb.reshape(128, -1)
    t['aown'] = ao.reshape(128, -1)
    t['btab'] = bt.reshape(128, -1)
    p = np.arange(128)[:, None]
    c = np.arange(T)[None, :]
    dm0 = np.zeros((128, 4, T), np.float32)
    for i in range(4):
        k = i * 128 + p
        dm0[:, i, :] = np.where(((k // 256) == (c // 256)) & (k > c), -BIG, 0.0)
    t['dm0'] = dm0.astype(NPBF)
    return t


def _core_tables(hc):
    t = {}
    p = np.arange(128)[:, None]
    c = np.arange(T)[None, :]
    dm1 = np.zeros((128, 8, T), np.float32)
    for i in range(8):
        chunk_b = i >= 4
        k = (i % 4) * 128 + p
        own = (chunk_b == (hc == 1))
        if own:
            dm1[:, i, :] = np.where(k > c, -BIG, 0.0)
        else:
            dm1[:, i, :] = -BIG if (hc == 0 and chunk_b) else 0.0
    t['dm1'] = dm1.astype(NPBF)
    s2 = np.zeros((128, 2), np.float32)
    s2[:, hc] = 1.0
    t['sel2'] = s2
    return t


def _prep(inputs):
    f = lambda a: np.ascontiguousarray(np.asarray(a, dtype=np.float32))
    x = f(inputs['x'])
    p = f(inputs['p'])
    common = _consts()
    common.update(_moba_tables())
    g = np.stack([f(inputs['attn_norm'])[0], f(inputs['attn_norm'])[1], f(inputs['ffn_norm'])[0],
                  f(inputs['ffn_norm'])[1], f(inputs['ple_norm'])[0], f(inputs['ple_norm'])[1],
                  f(inputs['final_norm'])], axis=0)
    common['gains'] = np.ascontiguousarray(g.reshape(7, 8, 128).transpose(2, 0, 1))
    wqkv = f(inputs['moba_wqkv'])[0]
    wq, wk, wv = wqkv[:, 0:1024], wqkv[:, 1024:2048], wqkv[:, 2048:3072]
    common['wq0'] = np.ascontiguousarray(wq)
    common['wq0s'] = _swap_halves(wq, 8, 128)
    common['wk0'] = np.ascontiguousarray(wk)
    common['wk0s'] = _swap_halves(wk, 8, 128)
    common['wv0'] = np.ascontiguousarray(wv)
    common['wo0'] = f(inputs['moba_wo'])[0]
    common['wdq'] = f(inputs['mla_wdq'])[0]
    wuq = f(inputs['mla_wuq'])[0].reshape(384, 8, 192)
    common['wuqn'] = np.ascontiguousarray(wuq[:, :, :128].reshape(384, 1024))
    wr_ = np.ascontiguousarray(wuq[:, :, 128:].reshape(384, 512))
    common['wuqr'] = wr_
    common['wuqrs'] = _swap_halves(wr_, 8, 64)
    wdkv = f(inputs['mla_wdkv'])[0]
    common['wdkvc'] = np.ascontiguousarray(wdkv[:, :256])
    common['wdkvr'] = np.ascontiguousarray(wdkv[:, 256:])
    common['wdkvrs'] = _swap_halves(np.ascontiguousarray(wdkv[:, 256:]), 1, 64)
    common['qn'] = np.ascontiguousarray(f(inputs['mla_qnorm'])[0].reshape(3, 128).T)
    common['kvn'] = np.ascontiguousarray(f(inputs['mla_kvnorm'])[0].reshape(2, 128).T)
    wukv = f(inputs['mla_wukv'])[0].reshape(256, 8, 256)
    common['wukvk'] = np.ascontiguousarray(wukv[:, :, :128].reshape(256, 1024))
    common['wukvv'] = np.ascontiguousarray(wukv[:, :, 128:].reshape(256, 1024))
    common['wo1'] = f(inputs['mla_wo'])[0]
    for li in range(2):
        common[f'wr{li}'] = np.ascontiguousarray(
            np.concatenate([f(inputs['moe_wgroup'])[li], f(inputs['moe_wexpert'])[li]], axis=1))
        common[f'wg{li}'] = f(inputs['moe_wgate'])[li]
        common[f'wu{li}'] = f(inputs['moe_wup'])[li]
        common[f'wd{li}'] = f(inputs['moe_wdown'])[li]
        common[f'pg{li}'] = f(inputs['ple_gate'])[li]
        common[f'pp{li}'] = f(inputs['ple_proj'])[li]
    cosg, sing = _rope_tables(np.arange(NG), 128)
    common['cosg'], common['sing'] = cosg, sing
    c64g, s64g = _rope_tables(np.arange(NG), 64)
    common['cos64g'] = np.ascontiguousarray(np.concatenate([c64g, c64g], axis=0))
    common['sin64g'] = np.ascontiguousarray(np.concatenate([s64g, s64g], axis=0))
    tabs = [_core_tables(0), _core_tables(1)]
    locs = []
    per_core = []
    xT = [np.ascontiguousarray(x[b].T) for b in range(4)]
    p0T = [np.ascontiguousarray(p[0, b].T) for b in range(4)]
    for c in range(8):
        b, hc = c // 2, c % 2
        loc = np.concatenate([np.arange((2 * j + hc) * T, (2 * j + hc + 1) * T) for j in range(NJ)])
        locs.append(loc)
        d = dict(common)
        d.update(tabs[hc])
        d['xT_all'] = xT[b]
        d['p0T'] = p0T[b]
        d['p1T'] = np.ascontiguousarray(p[1, b][loc].T)
        c64, s64 = _rope_tables(loc, 64)
        d['cos64'] = np.ascontiguousarray(np.concatenate([c64, c64], axis=0))
        d['sin64'] = np.ascontiguousarray(np.concatenate([s64, s64], axis=0))
        per_core.append(d)
    return per_core, locs


_NC_CACHE = {}


def _get_nc(mode):
    if mode not in _NC_CACHE:
        _NC_CACHE[mode] = build(mode)
    return _NC_CACHE[mode]


def kernel(**inputs):
    per_core, locs = _prep(inputs)
    nc = _get_nc('fused')
    res = run_bass_kernel_spmd(nc, per_core, core_ids=list(range(8)))
    out = np.empty((4, NG, 1024), np.float32)
    for c in range(8):
        out[c // 2][locs[c]] = np.asarray(res.results[c]['outT'], dtype=np.float32).T
    return out
```
